# Optimizing a Trainium2 kernel written in Bass

```python
import jax, jax.numpy as jnp
from jax import lax
import numpy as np


D_MODEL = 1024
BATCH = 4
SEQ = 4096
DEPTH = 2

CONV_CH = D_MODEL
CONV_K = 31
SG_GROUPS = 8
SG_CH = D_MODEL
SG_DG = SG_CH // SG_GROUPS
CHUNK = 128
N_IN = 2 * CONV_CH + 2 * SG_CH + 2 * D_MODEL
FF_DENSE = 2816
N_EXPERTS = 8
TOP_K = 2
FF_EXPERT = 3584
N_DENSE = (DEPTH + 1) // 2
N_MOE = DEPTH // 2
EPS = 1e-6

kernel_name = "hybrid_conv_gmlp_gated_moe_block"


def rmsnorm(x, g):
    xf = x.astype(jnp.float32)
    y = xf * lax.rsqrt(jnp.mean(xf * xf, axis=-1, keepdims=True) + EPS)
    return (y * g.astype(jnp.float32)).astype(x.dtype)


def layernorm(x, g, b):
    xf = x.astype(jnp.float32)
    mu = jnp.mean(xf, axis=-1, keepdims=True)
    var = jnp.mean(jnp.square(xf - mu), axis=-1, keepdims=True)
    y = (xf - mu) * lax.rsqrt(var + EPS)
    return (y * g.astype(jnp.float32) + b.astype(jnp.float32)).astype(x.dtype)


def conv_branch(a_val, a_gate, conv_w, conv_b, ln_g, ln_b, w_out):
    a = a_val * jax.nn.sigmoid(a_gate)
    a = lax.conv_general_dilated(
        a, conv_w[:, None, :], window_strides=(1,),
        padding=[(CONV_K - 1, 0)],
        dimension_numbers=("NWC", "WIO", "NWC"),
        feature_group_count=CONV_CH) + conv_b
    a = jax.nn.silu(layernorm(a, ln_g, ln_b))
    return a @ w_out


def sgu_branch(u, v, ln_g, ln_b, w_s, b_s, w_out):
    bsz, seq, _ = u.shape
    u = jax.nn.gelu(u, approximate=False)
    v = jax.nn.gelu(v, approximate=False)
    v = v.reshape(bsz, seq // CHUNK, CHUNK, SG_GROUPS, SG_DG)
    v = layernorm(v, ln_g.reshape(SG_GROUPS, SG_DG), ln_b.reshape(SG_GROUPS, SG_DG))
    mask = jnp.tril(jnp.ones((CHUNK, CHUNK), dtype=bool))
    w = jnp.where(mask[None], w_s, jnp.zeros_like(w_s))
    v = jnp.einsum('gts,bnsgd->bntgd', w, v) + b_s.T[:, :, None]
    y = u * v.reshape(bsz, seq, SG_CH)
    return y @ w_out


def swiglu(h, w1, w3, w2):
    return (jax.nn.silu(h @ w1) * (h @ w3)) @ w2


def moe_ffn(h, router, w1, w3, w2):
    bsz, seq, d = h.shape
    t = h.reshape(bsz * seq, d)
    logits = t.astype(jnp.float32) @ router.astype(jnp.float32)
    top_vals, top_idx = lax.top_k(logits, TOP_K)
    top_w = jax.nn.softmax(top_vals, axis=-1)
    combine = jnp.sum(jax.nn.one_hot(top_idx, N_EXPERTS, dtype=jnp.float32)
                      * top_w[..., None], axis=1).astype(h.dtype)
    out = jnp.zeros_like(t)
    for e in range(N_EXPERTS):
        out = out + combine[:, e:e + 1] * swiglu(t, w1[e], w3[e], w2[e])
    return out.reshape(bsz, seq, d)


def setup_inputs(seed: int = 0) -> dict:
    key = jax.random.key(seed)
    ks = jax.random.split(key, 32)
    f32 = jnp.float32
    D = D_MODEL

    def nrm(k, shape, scale):
        return jax.random.normal(k, shape, f32) * scale

    def gain(k, shape):
        return 1.0 + 0.01 * jax.random.normal(k, shape, f32)

    return {
        "x": nrm(ks[0], (BATCH, SEQ, D), 1.0),
        "g_mix": gain(ks[1], (DEPTH, D)),
        "w_in": nrm(ks[2], (DEPTH, D, N_IN), D ** -0.5),
        "conv_w": nrm(ks[3], (DEPTH, CONV_K, CONV_CH), CONV_K ** -0.5),
        "conv_b": nrm(ks[4], (DEPTH, CONV_CH), 0.01),
        "conv_ln_g": gain(ks[5], (DEPTH, CONV_CH)),
        "conv_ln_b": nrm(ks[6], (DEPTH, CONV_CH), 0.01),
        "w_conv_out": nrm(ks[7], (DEPTH, CONV_CH, D), CONV_CH ** -0.5),
        "sg_ln_g": gain(ks[8], (DEPTH, SG_CH)),
        "sg_ln_b": nrm(ks[9], (DEPTH, SG_CH), 0.01),
        "sg_w": nrm(ks[10], (DEPTH, SG_GROUPS, CHUNK, CHUNK), CHUNK ** -0.5),
        "sg_b": gain(ks[11], (DEPTH, SG_GROUPS, CHUNK)),
        "w_sg_out": nrm(ks[12], (DEPTH, SG_CH, D), SG_CH ** -0.5),
        "w_o": nrm(ks[13], (DEPTH, D, D), D ** -0.5),
        "g_ffn": gain(ks[14], (DEPTH, D)),
        "ffn_w1": nrm(ks[15], (N_DENSE, D, FF_DENSE), D ** -0.5),
        "ffn_w3": nrm(ks[16], (N_DENSE, D, FF_DENSE), D ** -0.5),
        "ffn_w2": nrm(ks[17], (N_DENSE, FF_DENSE, D), FF_DENSE ** -0.5),
        "moe_router": nrm(ks[18], (N_MOE, D, N_EXPERTS), D ** -0.5),
        "moe_w1": nrm(ks[19], (N_MOE, N_EXPERTS, D, FF_EXPERT), D ** -0.5),
        "moe_w3": nrm(ks[20], (N_MOE, N_EXPERTS, D, FF_EXPERT), D ** -0.5),
        "moe_w2": nrm(ks[21], (N_MOE, N_EXPERTS, FF_EXPERT, D), FF_EXPERT ** -0.5),
        "g_final": gain(ks[22], (D,)),
    }


def reference(x, g_mix, w_in, conv_w, conv_b, conv_ln_g, conv_ln_b, w_conv_out,
              sg_ln_g, sg_ln_b, sg_w, sg_b, w_sg_out, w_o, g_ffn,
              ffn_w1, ffn_w3, ffn_w2, moe_router, moe_w1, moe_w3, moe_w2, g_final):
    o_ag = CONV_CH
    o_u = 2 * CONV_CH
    o_v = o_u + SG_CH
    o_ga = o_v + SG_CH
    o_gb = o_ga + D_MODEL
    for l in range(DEPTH):
        h = rmsnorm(x, g_mix[l])
        p = h @ w_in[l]
        ya = conv_branch(p[..., :o_ag], p[..., o_ag:o_u], conv_w[l], conv_b[l],
                         conv_ln_g[l], conv_ln_b[l], w_conv_out[l])
        yb = sgu_branch(p[..., o_u:o_v], p[..., o_v:o_ga], sg_ln_g[l], sg_ln_b[l],
                        sg_w[l], sg_b[l], w_sg_out[l])
        m = jax.nn.sigmoid(p[..., o_ga:o_gb]) * ya + jax.nn.sigmoid(p[..., o_gb:]) * yb
        x = x + m @ w_o[l]
        h = rmsnorm(x, g_ffn[l])
        j = l // 2
        if l % 2 == 0:
            x = x + swiglu(h, ffn_w1[j], ffn_w3[j], ffn_w2[j])
        else:
            x = x + moe_ffn(h, moe_router[j], moe_w1[j], moe_w3[j], moe_w2[j])
    return rmsnorm(x, g_final)
```

```python
import numpy as np
import concourse.bass as bass
import concourse.mybir as mybir
from concourse.bass_utils import run_bass_kernel_spmd

F32 = mybir.dt.float32
BF16 = mybir.dt.bfloat16
AF = mybir.ActivationFunctionType
ALU = mybir.AluOpType
AX = mybir.AxisListType

D = 1024
KT = 8
SEQ = 4096
BATCH = 4
NCORES = 8
TOK = 2048
NTOK = TOK + 128
CONV_K = 31
FF_DENSE = 2816
FF_EXPERT = 3584
NE = 8
EPS = 1e-6
NSLOT = 6
V_GMIX, V_CONVB, V_CLNG, V_CLNB, V_SLNG, V_SLNB, V_GFFN, V_GFIN = range(8)
NV = 8


class Res:
    __slots__ = ("name", "w", "r")

    def __init__(self, name):
        self.name = name
        self.w = None
        self.r = {}


class Eng:
    def __init__(self, name, handle, sem):
        self.name = name
        self.h = handle
        self.sem = sem
        self.count = 0
        self.known = {}
        self.pending = False


class Prog:
    def __init__(self, nc, sems):
        self.nc = nc
        self.sems = sems
        self.eng = {}
        self.dma_count = {}
        self.trace = {}

    def add_engine(self, name, handle, semname):
        self.eng[name] = Eng(name, handle, semname)

    def _wait(self, e, toks):
        need = {}
        for t in toks:
            if t is None:
                continue
            k, v = t[0], t[1]
            if k == e.sem and (len(t) < 3 or e.name == "pe"):
                continue
            if e.known.get(k, 0) >= v:
                continue
            if need.get(k, 0) < v:
                need[k] = v
        for k, v in need.items():
            e.h.wait_ge(self.sems[k], v)
            e.known[k] = v
            self.trace.setdefault(e.name, []).append(("wait", k, v))

    def _deps(self, reads, writes):
        toks = []
        for r in reads:
            if r.w is not None:
                toks.append((r.w[0], r.w[1], "raw"))
        for w in writes:
            toks.append(w.w)
            for k, v in w.r.items():
                toks.append((k, v))
        return toks

    def _commit(self, tok, reads, writes):
        for r in reads:
            k, v = tok
            if r.r.get(k, 0) < v:
                r.r[k] = v
        for w in writes:
            w.w = tok
            w.r = {}

    def op(self, ename, fn, reads=(), writes=(), sig=True):
        e = self.eng[ename]
        self._wait(e, self._deps(reads, writes))
        ins = fn(e.h)
        if sig:
            e.count += 1
            ins.then_inc(self.sems[e.sem], 1)
            self.trace.setdefault(e.name, []).append(("inc", e.sem, 1))
            e.pending = False
            tok = (e.sem, e.count)
        else:
            e.pending = True
            tok = (e.sem, e.count + 1)
        self._commit(tok, reads, writes)
        return ins

    def dma(self, qname, semname, out, in_, reads=(), writes=()):
        e = self.eng[qname]
        self._wait(e, self._deps(reads, writes))
        e.h.dma_start(out=out, in_=in_).then_inc(self.sems[semname], 16)
        self.trace.setdefault(e.name, []).append(("inc", semname, 16))
        c = self.dma_count.get(semname, 0) + 16
        self.dma_count[semname] = c
        tok = (semname, c)
        self._commit(tok, reads, writes)
        return tok

    def dma_multi(self, qname, semname, pairs, reads=(), writes=()):
        e = self.eng[qname]
        self._wait(e, self._deps(reads, writes))
        for out, in_ in pairs:
            e.h.dma_start(out=out, in_=in_).then_inc(self.sems[semname], 16)
            self.trace.setdefault(e.name, []).append(("inc", semname, 16))
        c = self.dma_count.get(semname, 0) + 16 * len(pairs)
        self.dma_count[semname] = c
        tok = (semname, c)
        self._commit(tok, reads, writes)
        return tok

    def retoken(self, semname, ress):
        tok = (semname, self.dma_count[semname])
        for r in ress:
            r.w = tok

    def barrier(self, names=("pe", "act", "dve")):
        for n in names:
            assert not self.eng[n].pending, n
        for n in names:
            e = self.eng[n]
            toks = [(self.eng[m].sem, self.eng[m].count) for m in names if m != n]
            self._wait(e, toks)

    def check_deadlock(self):
        pos = {n: 0 for n in self.trace}
        val = {}
        progress = True
        while progress:
            progress = False
            for n, ev in self.trace.items():
                while pos[n] < len(ev):
                    kind, k, v = ev[pos[n]]
                    if kind == "wait":
                        if val.get(k, 0) < v:
                            break
                    else:
                        val[k] = val.get(k, 0) + v
                    pos[n] += 1
                    progress = True
        stuck = {n: (pos[n], len(ev), ev[pos[n]]) for n, ev in self.trace.items() if pos[n] < len(ev)}
        return stuck, val

    def wait_all(self, ename, toks):
        self._wait(self.eng[ename], toks)


DBG = [99]
DUMP = [0]
LASTRES = [None]


def build_nc(depth_run=2, debug_out=None):
    nc = bass.Bass("TRN2", target_bir_lowering=False)
    dt = nc.dram_tensor
    xT = dt("xT", [D, 2304], F32, kind="ExternalInput").ap()
    w_in = dt("w_in", [2, D, 6144], F32, kind="ExternalInput").ap()
    w_co = dt("w_conv_out", [2, D, D], F32, kind="ExternalInput").ap()
    w_so = dt("w_sg_out", [2, D, D], F32, kind="ExternalInput").ap()
    w_o = dt("w_o", [2, D, D], F32, kind="ExternalInput").ap()
    f_w1 = dt("ffn_w1", [D, FF_DENSE], F32, kind="ExternalInput").ap()
    f_w3 = dt("ffn_w3", [D, FF_DENSE], F32, kind="ExternalInput").ap()
    f_w2 = dt("ffn_w2", [FF_DENSE, D], F32, kind="ExternalInput").ap()
    if depth_run >= 2:
        m_w1 = dt("moe_w1", [NE, D, FF_EXPERT], F32, kind="ExternalInput").ap()
        m_w3 = dt("moe_w3", [NE, D, FF_EXPERT], F32, kind="ExternalInput").ap()
        m_w2 = dt("moe_w2", [NE, FF_EXPERT, D], F32, kind="ExternalInput").ap()
    vecs_d = dt("vecs", [128, 2, NV, KT], F32, kind="ExternalInput").ap()
    convw_d = dt("convw", [128, 2, KT, CONV_K], F32, kind="ExternalInput").ap()
    sgwT_d = dt("sgwT", [128, 2, 8, 128], F32, kind="ExternalInput").ap()
    sgb_d = dt("sgb", [128, 2, 8, 128], F32, kind="ExternalInput").ap()
    router_d = dt("router", [128, KT, NE], F32, kind="ExternalInput").ap()
    tril_d = dt("tril", [128, 128], F32, kind="ExternalInput").ap()
    ident_d = dt("ident", [128, 128], F32, kind="ExternalInput").ap()
    cmask_d = dt("cmask", [128, 1], F32, kind="ExternalInput").ap()
    yT = dt("yT", [D, TOK], F32, kind="ExternalOutput").ap()

    from contextlib import ExitStack
    with ExitStack() as es:
        def sb(name, shape, dtype):
            return es.enter_context(nc.sbuf_tensor(name, shape, dtype))

        semnames = ["pe", "act", "dve", "cs", "xs", "os", "x0s", "sg0", "sg1"] + [f"xb{k}" for k in range(KT)] + [f"ws{i}" for i in range(NSLOT)]
        sems = {n: es.enter_context(nc.semaphore(n)) for n in semnames}
        P = Prog(nc, sems)
        P.add_engine("pe", nc.tensor, "pe")
        P.add_engine("act", nc.scalar, "act")
        P.add_engine("dve", nc.vector, "dve")
        P.add_engine("sp", nc.sync, None)
        P.add_engine("pool", nc.gpsimd, None)

        x_all = sb("x_all", [128, KT, NTOK], F32)
        ring = sb("ring", [128, NSLOT, 4096], BF16)
        vecs = sb("vecs_sb", [128, 2, NV, KT], F32)
        convw = sb("convw_sb", [128, 2, KT, CONV_K], F32)
        sgw_f = sb("sgw_f", [128, 8, 128], F32)
        sgb = sb("sgb_sb", [128, 8, 128], F32)
        router = sb("router_sb", [128, KT, NE], F32)
        tril = sb("tril_sb", [128, 128], F32)
        ident = sb("ident_sb", [128, 128], F32)
        cmask = sb("cmask_sb", [128, 1], F32)
        ones_bf = sb("ones_bf", [128, 128], BF16)
        ones_f = sb("ones_f", [128, 128], F32)
        onecol = sb("onecol", [128, 1], F32)
        epscol = sb("epscol", [128, 1], F32)
        ident_bf = sb("ident_bf", [128, 128], BF16)
        sgwT = sb("sgwT_bf", [128, 8, 128], BF16)
        Cg = sb("Cg", [128, 8, 128], F32)

        psum = [es.enter_context(nc.psum_tensor(f"ps{i}", [128, 512], F32)) for i in range(8)]
        ps_res = [Res(f"ps{i}") for i in range(8)]
        ps_half = [[Res(f"ps{i}h{h}") for h in range(2)] for i in range(8)]
        bank_ctr = [0]

        def bres(b):
            return [ps_half[b][0], ps_half[b][1]]

        def next_bank(excl=()):
            while True:
                b = bank_ctr[0] % 8
                bank_ctr[0] += 1
                if b not in excl:
                    return b

        r_x = [[Res(f"x{k}_{c}") for c in range(17)] for k in range(KT)]
        r_const = Res("const")
        r_sgwT = Res("sgwT")
        r_Cg = Res("Cg")
        slot_res = [Res(f"slot{i}") for i in range(NSLOT)]
        panel_ctr = [0]

        r_sgin = Res("sgin")
        cl = [(vecs, vecs_d), (convw, convw_d), (router, router_d),
              (tril, tril_d), (ident, ident_d), (cmask, cmask_d)]
        for t, d_ in cl:
            P.dma("sp", "cs", t[:], d_, writes=[r_const])
        P.retoken("cs", [r_const])
        def load_x():
            P.dma("sp", "xs", x_all[:, :, 0:128], xT[:, 128:256].rearrange("(k p) c -> p k c", p=128),
                  writes=[r_x[k][0] for k in range(KT)])
            for k in range(KT):
                P.dma("sp", f"xb{k}", x_all[:, k, 128:NTOK], xT[k * 128:(k + 1) * 128, 256:2304],
                      writes=[r_x[k][c] for c in range(1, 17)])
        r_ones = Res("ones")
        P.op("dve", lambda e: e.memset(ones_bf[:], 1.0), writes=[r_ones])
        P.op("dve", lambda e: e.memset(ones_f[:], 1.0), writes=[r_ones])
        P.op("dve", lambda e: e.memset(onecol[:], 1.0), writes=[r_ones])
        P.op("dve", lambda e: e.memset(epscol[:], EPS), writes=[r_ones])
        P.op("dve", lambda e: e.tensor_copy(out=ident_bf[:], in_=ident[:]), reads=[r_const], writes=[r_ones])

        dumps = []

        def dump(name, ap, shape, dtype, reads):
            if not DUMP[0]:
                return
            d_ = nc.dram_tensor("dbg_" + name, list(shape), dtype, kind="ExternalOutput").ap()
            P.dma("sp", "os", d_, ap, reads=reads)
            dumps.append(name)

        def xs(k, t0, n):
            return x_all[:, k, t0:t0 + n]

        def xres(k, t0, n):
            return [r_x[k][c] for c in range(t0 // 128, (t0 + n) // 128)]

        def load_panel(src2d, kt, cols):
            i = panel_ctr[0] % NSLOT
            panel_ctr[0] += 1
            dst = ring[:, i, 0:kt * cols].rearrange("p (k n) -> p k n", k=kt)
            srcv = src2d.rearrange("(k p) n -> p k n", p=128)
            pairs = []
            for c0 in range(0, cols, 512):
                c1 = min(cols, c0 + 512)
                pairs.append((dst[:, :, c0:c1], srcv[:, :, c0:c1]))
            P.dma_multi("pool", f"ws{i}", pairs, writes=[slot_res[i]])
            return dst, slot_res[i]

        rms_ctr = [0]

        def rms_tile(src_fn, src_res_fn, n, gcol_fn, out_fn, out_res, sq, r_sq, stat, r_stat, extra_f32=None,
                     alt_rstd=False):
            b = next_bank()
            for k in range(KT):
                s = k % 2
                P.op("act", lambda e, k=k, s=s: e.activation(out=sq[:, s, 0:n], in_=src_fn(k), func=AF.Square),
                     reads=src_res_fn(k), writes=[r_sq[s]])
                P.op("pe", lambda e, k=k, s=s: e.matmul(psum[b][:, 0:n], lhsT=ones_bf[:], rhs=sq[:, s, 0:n],
                                                        start=(k == 0), stop=(k == KT - 1)),
                     reads=[r_sq[s], r_ones], writes=bres(b), sig=True)
            rslot = rms_ctr[0] % 2 if alt_rstd else 0
            rms_ctr[0] += 1
            rstd = stat[:, rslot, 0:n]
            P.op("act", lambda e: e.activation(out=rstd, in_=psum[b][:, 0:n], func=AF.Sqrt, bias=epscol[:, 0:1],
                                               scale=1.0 / D),
                 reads=bres(b) + [r_ones], writes=[r_stat[rslot]])
            P.op("dve", lambda e: e.reciprocal(out=rstd, in_=rstd),
                 reads=[r_stat[rslot]], writes=[r_stat[rslot]])
            for k in range(KT):
                P.op("dve", lambda e, k=k: e.scalar_tensor_tensor(out=out_fn(k), in0=src_fn(k), scalar=gcol_fn(k),
                                                                  in1=rstd, op0=ALU.mult, op1=ALU.mult),
                     reads=src_res_fn(k) + [r_stat[rslot], r_const], writes=[out_res[k]])
                if extra_f32 is not None:
                    ef, er = extra_f32
                    P.op("dve", lambda e, k=k: e.scalar_tensor_tensor(out=ef(k), in0=src_fn(k), scalar=gcol_fn(k),
                                                                      in1=rstd, op0=ALU.mult, op1=ALU.mult),
                         reads=src_res_fn(k) + [r_stat[rslot], r_const], writes=[er[k]])
            return rslot

        def mm_cols(bank, n, panel, pres, col0, rhs_fn, rhs_res, kt=KT, sig_last=True):
            for k in range(kt):
                P.op("pe", lambda e, k=k: e.matmul(psum[bank][:, 0:n], lhsT=panel[:, k, col0:col0 + 128],
                                                   rhs=rhs_fn(k), start=(k == 0), stop=(k == kt - 1)),
                     reads=[pres] + rhs_res(k), writes=bres(bank), sig=(sig_last and k == kt - 1))

        def mixer_layer(l):
            with ExitStack() as ms:
                def msb(name, shape, dtype):
                    return ms.enter_context(nc.sbuf_tensor(f"{name}_{l}", shape, dtype))
                h = msb("h", [128, KT, 512], BF16)
                a_ext = msb("a_ext", [128, KT, 30 + 512], BF16)
                cv = msb("cv", [128, KT, 512], F32)
                ma = msb("ma", [128, KT, 512], BF16)
                gu = msb("gu", [128, KT, 512], BF16)
                sq = msb("sq", [128, 4, 512], BF16)
                sg = msb("sg", [128, 2, 512], F32)
                stat = msb("stat", [128, 4, 512], F32)
                small = msb("small", [128, 16, 8], F32)
                dgA = msb("dgA", [128, 16, 128], BF16)
                dgB = msb("dgB", [128, 16, 128], BF16)
                r_dgA = Res("dgA")
                r_dgB = Res("dgB")
                r_h = [Res(f"h{k}") for k in range(KT)]
                r_a = [Res(f"a{k}") for k in range(KT)]
                r_cv = [Res(f"cv{k}") for k in range(KT)]
                r_ma = [Res(f"ma{k}") for k in range(KT)]
                r_gu = [Res(f"gu{k}") for k in range(KT)]
                r_sq = [Res(f"sq{k}") for k in range(4)]
                r_sg = [Res(f"sg{k}") for k in range(2)]
                r_stat = [Res(f"stat{k}") for k in range(4)]
                x0 = cv[:, 0:2, :].rearrange("p a (b c) -> p (a b) c", c=128)
                r_x0 = [r_cv[0]] * 4 + [r_cv[1]] * 4
                r_small = Res("small")
                sg_ctr = [0]
                gvs = [cv[:, 0:2, :].rearrange("p a b -> p (a b)"),
                       cv[:, 2:4, :].rearrange("p a b -> p (a b)")]
                gv = gvs[0]
                sqv = dgA[:, :, :].rearrange("p a b -> p (a b)").bitcast(F32)
                vn = cv[:, 4:8, :].rearrange("p a b -> p (a b)").bitcast(BF16).rearrange("p (c n) -> p c n", c=4)
                r_gvs = [[r_cv[0], r_cv[1]], [r_cv[2], r_cv[3]]]
                r_gv = r_gvs[0]
                r_sqv = [r_dgA]

                def vcol(idx, j):
                    return vecs[:, l, idx, j:j + 1]

                P.dma_multi("sp", f"sg{l}", [(sgw_f[:], sgwT_d[:, l, :, :]), (sgb[:], sgb_d[:, l, :, :])],
                            writes=[r_sgin])
                P.op("dve", lambda e: e.tensor_tensor(out=sgwT[:], in0=sgw_f[:, :, :],
                                                      in1=tril[:].unsqueeze(1).to_broadcast([128, 8, 128]),
                                                      op=ALU.mult),
                     reads=[r_const, r_sgin], writes=[r_sgwT])
                for half in range(2):
                    b = next_bank()
                    P.op("pe", lambda e, half=half, b=b: e.matmul(
                        psum[b][:, :], lhsT=ones_bf[:],
                        rhs=sgwT[:, half * 4:(half + 1) * 4, :].rearrange("p g t -> p (g t)"),
                        start=True, stop=True), reads=[r_sgwT, r_ones], writes=bres(b))
                    for gg in range(4):
                        g = half * 4 + gg
                        P.op("dve", lambda e, g=g, gg=gg, b=b: e.scalar_tensor_tensor(
                            out=Cg[:, g, :], in0=psum[b][:, gg * 128:(gg + 1) * 128], scalar=vcol(V_SLNB, g),
                            in1=sgb[:, g, :], op0=ALU.mult, op1=ALU.add),
                            reads=bres(b) + [r_const, r_sgin], writes=[r_Cg])

                if l == 0:
                    load_x()

                h_ready = [None]

                def m1(t0, n, use_x0=False):
                    if use_x0:
                        src_fn = lambda k: x0[:, k, 0:n]
                        src_res = lambda k: [r_x0[k]]
                    else:
                        src_fn = lambda k: xs(k, t0, n)
                        src_res = lambda k: xres(k, t0, n)
                    rms_tile(src_fn, src_res, n, lambda k: vcol(V_GMIX, k), lambda k: h[:, k, 0:n], r_h,
                             sq, r_sq, stat, r_stat)
                    h_ready[0] = (t0, n, use_x0)

                def tile(t0, n, a_only, use_x0=False, masked=False, nxt=None):
                    nch = n // 128
                    if h_ready[0] != (t0, n, use_x0):
                        m1(t0, n, use_x0)
                    h_ready[0] = None
                    hk = lambda k: h[:, k, 0:n]
                    hres = lambda k: [r_h[k]]
                    W = w_in[l]
                    mcol = cmask[:, 0:1] if masked else onecol[:, 0:1]

                    def build_diag(j):
                        P.op("dve", lambda e: e.tensor_tensor(
                            out=dgA[:, 0:16, :], in0=ident_bf[:].unsqueeze(1).to_broadcast([128, 16, 128]),
                            in1=convw[:, l, j, 0:16].unsqueeze(2).to_broadcast([128, 16, 128]), op=ALU.mult),
                            reads=[r_const, r_ones], writes=[r_dgA])
                        P.op("dve", lambda e: e.tensor_tensor(
                            out=dgB[:, 0:15, :], in0=ident_bf[:].unsqueeze(1).to_broadcast([128, 15, 128]),
                            in1=convw[:, l, j, 16:31].unsqueeze(2).to_broadcast([128, 15, 128]), op=ALU.mult),
                            reads=[r_const, r_ones], writes=[r_dgB])
                    if not a_only:
                        build_diag(0)
                    for jj in range(2):
                        pv, rv = load_panel(W[:, jj * 512:(jj + 1) * 512], KT, 512)
                        pg, rg = load_panel(W[:, 1024 + jj * 512:1024 + (jj + 1) * 512], KT, 512)
                        for j4 in range(4):
                            j = jj * 4 + j4
                            b1 = next_bank()
                            b2 = next_bank()
                            mm_cols(b1, n, pv, rv, j4 * 128, hk, hres)
                            mm_cols(b2, n, pg, rg, j4 * 128, hk, hres)
                            s = sg_ctr[0] % 2
                            sg_ctr[0] += 1
                            P.op("act", lambda e, s=s, b2=b2: e.activation(out=sg[:, s, 0:n], in_=psum[b2][:, 0:n],
                                                                          func=AF.Sigmoid),
                                 reads=bres(b2), writes=[r_sg[s]])
                            P.op("dve", lambda e, s=s, b1=b1, j=j: e.scalar_tensor_tensor(
                                out=a_ext[:, j, 30:30 + n], in0=psum[b1][:, 0:n], scalar=mcol, in1=sg[:, s, 0:n],
                                op0=ALU.mult, op1=ALU.mult),
                                reads=bres(b1) + [r_sg[s], r_const, r_ones], writes=[r_a[j]])
                    if a_only:
                        for j in range(KT):
                            P.op("act", lambda e, j=j: e.activation(out=a_ext[:, j, 0:30], in_=a_ext[:, j, n:n + 30],
                                                                    func=AF.Copy),
                                 reads=[r_a[j]], writes=[r_a[j]])
                        return
                    if DBG[0] <= 2:
                        return
                    bmu, bsq = 6, 7
                    pu = ru = None
                    stat_pending = []
                    for j in range(KT):
                        b = next_bank(excl=(bmu, bsq))
                        if j > 0:
                            build_diag(j)
                        for k in range(CONV_K):
                            dgt, rdg, kk = (dgA, r_dgA, k) if k < 16 else (dgB, r_dgB, k - 16)
                            P.op("pe", lambda e, j=j, k=k, kk=kk, dgt=dgt, b=b: e.matmul(
                                psum[b][:, 0:n], lhsT=dgt[:, kk, :], rhs=a_ext[:, j, k:k + n],
                                start=(k == 0), stop=(k == CONV_K - 1)),
                                reads=[rdg, r_a[j]], writes=bres(b), sig=(k == 15 or k == CONV_K - 1))
                        while stat_pending:
                            stat_pending.pop(0)()
                        s0, s1 = (2 * j) % 4, (2 * j + 1) % 4
                        P.op("act", lambda e, j=j, b=b: e.activation(out=cv[:, j, 0:n], in_=psum[b][:, 0:n],
                                                                      func=AF.Identity, bias=vcol(V_CONVB, j),
                                                                      scale=1.0),
                             reads=bres(b) + [r_const], writes=[r_cv[j]])
                        P.op("act", lambda e, j=j, b=b, s0=s0: e.activation(out=sq[:, s0, 0:n], in_=psum[b][:, 0:n],
                                                                            func=AF.Identity,
                                                                            bias=vcol(V_CONVB, j), scale=1.0),
                             reads=bres(b) + [r_const], writes=[r_sq[s0]])
                        P.op("act", lambda e, j=j, b=b, s1=s1: e.activation(out=sq[:, s1, 0:n], in_=psum[b][:, 0:n],
                                                                            func=AF.Square,
                                                                            bias=vcol(V_CONVB, j), scale=1.0),
                             reads=bres(b) + [r_const], writes=[r_sq[s1]])
                        def stat_mm(j=j, s0=s0, s1=s1):
                            P.op("pe", lambda e: e.matmul(psum[bmu][:, 0:n], lhsT=ones_bf[:],
                                                          rhs=sq[:, s0, 0:n], start=(j == 0), stop=(j == KT - 1)),
                                 reads=[r_sq[s0], r_ones], writes=bres(bmu))
                            P.op("pe", lambda e: e.matmul(psum[bsq][:, 0:n], lhsT=ones_bf[:],
                                                          rhs=sq[:, s1, 0:n], start=(j == 0), stop=(j == KT - 1)),
                                 reads=[r_sq[s1], r_ones], writes=bres(bsq))
                        stat_pending.append(stat_mm)
                        P.op("act", lambda e, j=j: e.activation(out=a_ext[:, j, 0:30], in_=a_ext[:, j, n:n + 30],
                                                                func=AF.Copy),
                             reads=[r_a[j]], writes=[r_a[j]])
                    if DBG[0] <= 3:
                        return
                    mean = stat[:, 1, 0:n]
                    var = stat[:, 2, 0:n]

                    def ln_head():
                        P.op("dve", lambda e: e.tensor_scalar(out=mean, in0=psum[bmu][:, 0:n], scalar1=1.0 / D,
                                                              scalar2=None, op0=ALU.mult),
                             reads=bres(bmu), writes=[r_stat[1]])
                        P.op("dve", lambda e: e.tensor_tensor(out=var, in0=mean, in1=mean, op=ALU.mult),
                             reads=[r_stat[1]], writes=[r_stat[2]])
                        P.op("dve", lambda e: e.scalar_tensor_tensor(out=var, in0=psum[bsq][:, 0:n], scalar=1.0 / D,
                                                                     in1=var, op0=ALU.mult, op1=ALU.subtract),
                             reads=bres(bsq) + [r_stat[2]], writes=[r_stat[2]])
                        for j in range(KT):
                            P.op("dve", lambda e, j=j: e.tensor_tensor(out=cv[:, j, 0:n], in0=cv[:, j, 0:n],
                                                                       in1=mean, op=ALU.subtract),
                                 reads=[r_cv[j], r_stat[1]], writes=[r_cv[j]])

                    upend = stat_pending
                    for j in range(KT):
                        if j % 4 == 0:
                            pu, ru = load_panel(W[:, 2048 + (j // 4) * 512:2048 + (j // 4 + 1) * 512], KT, 512)
                        bu = next_bank(excl=(bmu, bsq))
                        mm_cols(bu, n, pu, ru, (j % 4) * 128, hk, hres)
                        if j == 0:
                            while upend:
                                upend.pop(0)()
                            ln_head()
                        P.op("act", lambda e, bu=bu, j=j: e.activation(out=gu[:, j, 0:n], in_=psum[bu][:, 0:n],
                                                                      func=AF.Gelu),
                             reads=bres(bu), writes=[r_gu[j]])
                        if j == 0:
                            P.op("act", lambda e: e.activation(out=var, in_=var, func=AF.Sqrt, bias=epscol[:, 0:1],
                                                               scale=1.0),
                                 reads=[r_stat[2], r_ones], writes=[r_stat[2]])
                            P.op("dve", lambda e: e.reciprocal(out=var, in_=var),
                                 reads=[r_stat[2]], writes=[r_stat[2]])
                            for jn in range(KT):
                                P.op("dve", lambda e, jn=jn: e.tensor_tensor(out=cv[:, jn, 0:n], in0=cv[:, jn, 0:n],
                                                                             in1=var, op=ALU.mult),
                                     reads=[r_cv[jn], r_stat[2]], writes=[r_cv[jn]])
                    for j in range(KT):
                        P.op("act", lambda e, j=j: e.activation(out=a_ext[:, j, 30:30 + n], in_=cv[:, j, 0:n],
                                                                func=AF.Silu, bias=vcol(V_CLNB, j),
                                                                scale=vcol(V_CLNG, j)),
                             reads=[r_cv[j], r_const], writes=[r_a[j]])
                    for jj in range(2):
                        pg, rg = load_panel(W[:, 4096 + jj * 512:4096 + (jj + 1) * 512], KT, 512)
                        for j4 in range(4):
                            j = jj * 4 + j4
                            b2 = next_bank(excl=(bmu, bsq))
                            mm_cols(b2, n, pg, rg, j4 * 128, hk, hres)
                            P.op("act", lambda e, b2=b2, j=j: e.activation(out=ma[:, j, 0:n], in_=psum[b2][:, 0:n],
                                                                          func=AF.Sigmoid),
                                 reads=bres(b2), writes=[r_ma[j]])
                    cak = lambda k: a_ext[:, k, 30:30 + n]
                    cares = lambda k: [r_a[k]]
                    if DBG[0] <= 4:
                        return
                    for jj in range(2):
                        pc, rc = load_panel(w_co[l][:, jj * 512:(jj + 1) * 512], KT, 512)
                        for j4 in range(4):
                            j = jj * 4 + j4
                            b1 = next_bank()
                            mm_cols(b1, n, pc, rc, j4 * 128, cak, cares)
                            P.op("dve", lambda e, b1=b1, j=j: e.tensor_tensor(
                                out=ma[:, j, 0:n], in0=psum[b1][:, 0:n], in1=ma[:, j, 0:n], op=ALU.mult),
                                reads=bres(b1) + [r_ma[j]], writes=[r_ma[j]])
                    if DBG[0] <= 5:
                        return
                    if t0 == 128 and l == 0:
                        dump("h", h[:], [128, KT, 512], BF16, r_h)
                        dump("ca", a_ext[:], [128, KT, 542], BF16, r_a)
                        dump("ma1", ma[:], [128, KT, 512], BF16, r_ma)
                        dump("gu", gu[:], [128, KT, 512], BF16, r_gu)
                    if DBG[0] <= 6:
                        return
                    pv0, rv0 = load_panel(W[:, 3072:3584], KT, 512)
                    pv1, rv1 = load_panel(W[:, 3584:4096], KT, 512)
                    def v_stage_a(c):
                        gvc, r_gvc, so = gvs[c % 2], r_gvs[c % 2], 8 * (c % 2)
                        for half, (pv, rv) in enumerate(((pv0, rv0), (pv1, rv1))):
                            b1 = next_bank()
                            for k in range(KT):
                                P.op("pe", lambda e, k=k, b1=b1, pv=pv: e.matmul(
                                    psum[b1][:, :], lhsT=h[:, k, c * 128:(c + 1) * 128], rhs=pv[:, k, :],
                                    start=(k == 0), stop=(k == KT - 1)),
                                    reads=[rv, r_h[k]], writes=bres(b1), sig=(k == KT - 1))
                            P.op("act", lambda e, b1=b1, half=half: e.activation(
                                out=gvc[:, half * 512:(half + 1) * 512], in_=psum[b1][:, :], func=AF.Gelu),
                                reads=bres(b1), writes=[r_gvc[half]])
                        P.op("act", lambda e: e.activation(out=sqv, in_=gvc, func=AF.Square),
                             reads=r_gvc, writes=r_sqv)
                        s1 = small[:, so + 0, :]
                        s2 = small[:, so + 1, :]
                        mu = small[:, so + 2, :]
                        rs = small[:, so + 3, :]
                        r_sm = r_smalls[c % 2]
                        P.op("dve", lambda e: e.tensor_reduce(out=s1, in_=gvc.rearrange("p (g d) -> p g d", g=8),
                                                              axis=AX.X, op=ALU.add),
                             reads=r_gvc, writes=[r_sm])
                        P.op("dve", lambda e: e.tensor_reduce(out=s2, in_=sqv.rearrange("p (g d) -> p g d", g=8),
                                                              axis=AX.X, op=ALU.add),
                             reads=r_sqv, writes=[r_sm])
                        P.op("dve", lambda e: e.tensor_scalar(out=mu, in0=s1, scalar1=1.0 / 128, scalar2=None,
                                                              op0=ALU.mult), reads=[r_sm], writes=[r_sm])
                        P.op("dve", lambda e: e.tensor_tensor(out=rs, in0=mu, in1=mu, op=ALU.mult),
                             reads=[r_sm], writes=[r_sm])
                        P.op("dve", lambda e: e.scalar_tensor_tensor(out=rs, in0=s2, scalar=1.0 / 128, in1=rs,
                                                                     op0=ALU.mult, op1=ALU.subtract),
                             reads=[r_sm], writes=[r_sm])

                    def v_stage_b(c):
                        gvc, r_gvc, so = gvs[c % 2], r_gvs[c % 2], 8 * (c % 2)
                        mu = small[:, so + 2, :]
                        rs = small[:, so + 3, :]
                        nb = small[:, so + 4, :]
                        r_sm = r_smalls[c % 2]
                        P.op("act", lambda e: e.activation(out=rs, in_=rs, func=AF.Sqrt, bias=epscol[:, 0:1],
                                                           scale=1.0),
                             reads=[r_sm, r_ones], writes=[r_sm])
                        P.op("dve", lambda e: e.reciprocal(out=rs, in_=rs),
                             reads=[r_sm], writes=[r_sm])
                        P.op("dve", lambda e: e.scalar_tensor_tensor(out=nb, in0=mu, scalar=-1.0, in1=rs,
                                                                     op0=ALU.mult, op1=ALU.mult),
                             reads=[r_sm], writes=[r_sm])
                        P.op("dve", lambda e: e.tensor_tensor(
                            out=sqv.rearrange("p (g d) -> p g d", g=8), in0=gvc.rearrange("p (g d) -> p g d", g=8),
                            in1=rs.unsqueeze(2).to_broadcast([128, 8, 128]), op=ALU.mult),
                            reads=r_gvc + [r_sm], writes=r_sqv)
                        P.op("dve", lambda e: e.tensor_tensor(
                            out=vn[:, c, :].rearrange("p (g d) -> p g d", g=8),
                            in0=sqv.rearrange("p (g d) -> p g d", g=8),
                            in1=nb.unsqueeze(2).to_broadcast([128, 8, 128]), op=ALU.add),
                            reads=r_sqv + [r_sm], writes=[r_cv[4 + c]])

                    r_smalls = [Res("small0"), Res("small1")]
                    for c in range(nch + 1):
                        if c < nch:
                            v_stage_a(c)
                        if c >= 1:
                            v_stage_b(c - 1)
                    for jj in range(2):
                        pg, rg = load_panel(W[:, 5120 + jj * 512:5120 + (jj + 1) * 512], KT, 512)
                        for j4 in range(4):
                            j = jj * 4 + j4
                            b2 = next_bank()
                            mm_cols(b2, n, pg, rg, j4 * 128, hk, hres)
                            P.op("act", lambda e, b2=b2, j=j: e.activation(out=a_ext[:, j, 30:30 + n],
                                                                          in_=psum[b2][:, 0:n], func=AF.Sigmoid),
                                 reads=bres(b2), writes=[r_a[j]])
                    if t0 == 128 and l == 0:
                        dump("vn", vn, [128, 4, 1024], BF16, r_cv[4:8])
                        dump("gv", gv, [128, 1024], F32, r_gv)
                        dump("small", small[:], [128, 8, 8], F32, [r_small])
                    if DBG[0] <= 7:
                        return
                    for g in range(8):
                        b1 = next_bank()
                        for c in range(nch):
                            P.op("pe", lambda e, g=g, c=c, b1=b1: e.matmul(
                                psum[b1][:, c * 128:(c + 1) * 128], lhsT=vn[:, c, g * 128:(g + 1) * 128],
                                rhs=sgwT[:, g, :], start=True, stop=True),
                                reads=[r_cv[4 + c], r_sgwT], writes=bres(b1), sig=(c == nch - 1))
                        tmp = stat[:, 3, 0:n]
                        P.op("dve", lambda e, g=g, b1=b1: e.scalar_tensor_tensor(
                            out=tmp.rearrange("p (c t) -> p c t", c=nch),
                            in0=psum[b1][:, 0:n].rearrange("p (c t) -> p c t", c=nch),
                            scalar=vcol(V_SLNG, g),
                            in1=Cg[:, g, :].unsqueeze(1).to_broadcast([128, nch, 128]),
                            op0=ALU.mult, op1=ALU.add),
                            reads=bres(b1) + [r_Cg, r_const], writes=[r_stat[3]])
                        P.op("dve", lambda e, g=g: e.tensor_tensor(out=gu[:, g, 0:n], in0=gu[:, g, 0:n], in1=tmp,
                                                                   op=ALU.mult),
                             reads=[r_gu[g], r_stat[3]], writes=[r_gu[g]])
                    yk = lambda k: gu[:, k, 0:n]
                    yres = lambda k: [r_gu[k]]
                    if t0 == 128 and l == 0:
                        dump("y", gu[:], [128, KT, 512], BF16, r_gu)
                        dump("Cg", Cg[:], [128, 8, 128], F32, [r_Cg])
                    if DBG[0] <= 8:
                        return
                    for jj in range(2):
                        pc, rc = load_panel(w_so[l][:, jj * 512:(jj + 1) * 512], KT, 512)
                        for j4 in range(4):
                            j = jj * 4 + j4
                            b1 = next_bank()
                            mm_cols(b1, n, pc, rc, j4 * 128, yk, yres)
                            s = sg_ctr[0] % 2
                            sg_ctr[0] += 1
                            P.op("dve", lambda e, s=s, b1=b1, j=j: e.tensor_tensor(
                                out=sg[:, s, 0:n], in0=psum[b1][:, 0:n], in1=a_ext[:, j, 30:30 + n], op=ALU.mult),
                                reads=bres(b1) + [r_a[j]], writes=[r_sg[s]])
                            P.op("dve", lambda e, s=s, j=j: e.tensor_tensor(
                                out=ma[:, j, 0:n], in0=ma[:, j, 0:n], in1=sg[:, s, 0:n], op=ALU.add),
                                reads=[r_ma[j], r_sg[s]], writes=[r_ma[j]])
                    if t0 == 128 and l == 0:
                        dump("m", ma[:], [128, KT, 512], BF16, r_ma)
                    if DBG[0] <= 9:
                        return
                    if nxt is not None:
                        m1(nxt[0], nxt[1])
                    mk = lambda k: ma[:, k, 0:n]
                    mres = lambda k: [r_ma[k]]
                    for jj in range(2):
                        po, ro = load_panel(w_o[l][:, jj * 512:(jj + 1) * 512], KT, 512)
                        for j4 in range(4):
                            j = jj * 4 + j4
                            b1 = next_bank()
                            mm_cols(b1, n, po, ro, j4 * 128, mk, mres)
                            P.op("dve", lambda e, b1=b1, j=j: e.tensor_tensor(
                                out=xs(j, t0, n), in0=psum[b1][:, 0:n], in1=xs(j, t0, n), op=ALU.add),
                                reads=bres(b1) + xres(j, t0, n), writes=xres(j, t0, n))

                if l == 0:
                    for j in range(KT):
                        P.op("dve", lambda e, j=j: e.memset(a_ext[:, j, 0:30], 0.0), writes=[r_a[j]])
                    tile(0, 128, False)
                else:
                    tile(0, 128, True, masked=True)
                for i in range(4):
                    tile(128 + i * 512, 512, False, nxt=((128 + (i + 1) * 512, 512) if i < 3 else None))
                P.barrier()

        def ffn_layer(l, moe):
            with ExitStack() as ms:
                def msb(name, shape, dtype):
                    return ms.enter_context(nc.sbuf_tensor(f"f{name}_{l}", shape, dtype))
                h2 = msb("h2", [128, KT, NTOK], BF16)
                sq = msb("sq", [128, 4, 256], BF16)
                stat = msb("stat", [128, 2, 256], F32)
                ssl = msb("ssl", [128, 4, 256], F32)
                act = msb("act", [128, 8, 256], BF16)
                r_h2 = [[Res(f"h2_{k}_{c}") for c in range(17)] for k in range(KT)]
                r_sq = [Res(f"fsq{k}") for k in range(4)]
                r_stat = [Res(f"fstat{k}") for k in range(2)]
                r_ssl = [Res(f"ssl{k}") for k in range(4)]
                r_act = [Res(f"act{k}") for k in range(8)]
                if moe:
                    diagbuf = msb("diagbuf", [128, TOK], F32)
                    router_g = msb("router_g", [128, KT, NE], F32)
                    rtok = msb("rtok", [128, 16], F32)
                    r_rg = Res("router_g")
                    r_rtok = Res("rtok")
                    comb_b = msb("comb_b", [128, TOK], F32)
                    L = msb("L", [128, 16, NE], F32)
                    L2 = msb("L2", [128, 16, NE], F32)
                    comb = msb("comb", [128, 16, NE], F32)
                    m12 = msb("m12", [128, 4, 16], F32)
                    r_h2f = [Res("diagbuf")]
                    r_combb = Res("comb_b")
                    r_L = Res("L")
                    diag = diagbuf[:, :]
                    P.op("dve", lambda e: e.tensor_tensor(
                        out=router_g[:], in0=router[:],
                        in1=vecs[:, l, V_GFFN, :].unsqueeze(2).to_broadcast([128, KT, NE]), op=ALU.mult),
                        reads=[r_const], writes=[r_rg])
                tiles = ([] if moe else [(0, 128)]) + [(128 + 256 * i, 256) for i in range(8)]

                for (t0, n) in tiles:
                    out_res = [Res("tmp") for _ in range(KT)]
                    rslot = rms_tile(lambda k: xs(k, t0, n), lambda k: xres(k, t0, n), n,
                                     lambda k: vecs[:, l, V_GFFN, k:k + 1],
                                     lambda k: h2[:, k, t0:t0 + n], out_res, sq, r_sq, stat, r_stat, alt_rstd=True)
                    for k in range(KT):
                        for c in range(t0 // 128, (t0 + n) // 128):
                            r_h2[k][c].w = out_res[k].w
                    if moe:
                        for cc in range(n // 128):
                            c = (t0 - 128) // 128 + cc
                            tc0 = t0 + cc * 128
                            b = next_bank()
                            for k in range(KT):
                                P.op("pe", lambda e, k=k, tc0=tc0, b=b: e.matmul(
                                    psum[b][:, 0:NE], lhsT=x_all[:, k, tc0:tc0 + 128], rhs=router_g[:, k, :],
                                    start=(k == 0), stop=(k == KT - 1)),
                                    reads=xres(k, tc0, 128) + [r_rg], writes=bres(b), sig=(k == KT - 1))
                            P.op("pe", lambda e, cc=cc, b=b, rslot=rslot: e.matmul(
                                psum[b][:, NE:NE + 1], lhsT=stat[:, rslot, cc * 128:(cc + 1) * 128],
                                rhs=ident[:, 0:1], start=True, stop=True),
                                reads=[r_stat[rslot], r_const], writes=bres(b))
                            P.op("act", lambda e, c=c, b=b: e.activation(out=rtok[:, c:c + 1],
                                                                         in_=psum[b][:, NE:NE + 1], func=AF.Copy),
                                 reads=bres(b), writes=[r_rtok])
                            P.op("dve", lambda e, c=c, b=b: e.tensor_scalar(
                                out=L[:, c, :], in0=psum[b][:, 0:NE], scalar1=rtok[:, c:c + 1], scalar2=None,
                                op0=ALU.mult),
                                reads=bres(b) + [r_rtok], writes=[r_L])
                if moe:
                    m1 = m12[:, 0, :]
                    m2 = m12[:, 1, :]
                    den = m12[:, 2, :]

                    def bc(a):
                        return a.unsqueeze(2).to_broadcast([128, 16, NE])
                    ops = [
                        lambda e: e.tensor_reduce(out=m1, in_=L[:], axis=AX.X, op=ALU.max),
                        lambda e: e.tensor_tensor(out=L2[:], in0=L[:], in1=bc(m1), op=ALU.is_equal),
                        lambda e: e.scalar_tensor_tensor(out=L2[:], in0=L2[:], scalar=-1e30, in1=L[:],
                                                         op0=ALU.mult, op1=ALU.add),
                        lambda e: e.tensor_reduce(out=m2, in_=L2[:], axis=AX.X, op=ALU.max),
                        lambda e: e.tensor_tensor(out=L2[:], in0=L[:], in1=bc(m2), op=ALU.is_ge),
                        lambda e: e.tensor_tensor(out=comb[:], in0=L[:], in1=bc(m1), op=ALU.subtract),
                    ]
                    for f in ops:
                        P.op("dve", f, reads=[r_L], writes=[r_L])
                    P.op("act", lambda e: e.activation(out=comb[:], in_=comb[:], func=AF.Exp),
                         reads=[r_L], writes=[r_L])
                    ops = [
                        lambda e: e.tensor_tensor(out=comb[:], in0=comb[:], in1=L2[:], op=ALU.mult),
                        lambda e: e.tensor_reduce(out=den, in_=comb[:], axis=AX.X, op=ALU.add),
                        lambda e: e.reciprocal(out=den, in_=den),
                        lambda e: e.tensor_tensor(out=comb[:], in0=comb[:], in1=bc(den), op=ALU.mult),
                    ]
                    for f in ops:
                        P.op("dve", f, reads=[r_L], writes=[r_L])

                nexp = NE if moe else 1
                FF = FF_EXPERT if moe else FF_DENSE
                chunks = []
                f0 = 0
                while f0 < FF:
                    w = min(512, FF - f0)
                    chunks.append((f0, w))
                    f0 += w
                hslot = [0]
                pending = []
                if DBG[0] == 21:
                    chunks = []
                if DBG[0] == 22:
                    chunks = chunks[:1]
                if DBG[0] == 23:
                    chunks = chunks[-1:]
                if DBG[0] in (24, 25, 26, 27):
                    chunks = chunks[:1]
                    tiles = tiles[1:2]

                def flush_one():
                    if pending:
                        pending.pop(0)()

                for ex in range(nexp):
                    if moe:
                        while pending:
                            flush_one()
                        W1, W3, W2 = m_w1[ex], m_w3[ex], m_w2[ex]
                        P.op("dve", lambda e, ex=ex: e.tensor_tensor(
                            out=diag.rearrange("p (c t) -> p c t", c=16),
                            in0=ident[:].unsqueeze(1).to_broadcast([128, 16, 128]),
                            in1=comb[:, :, ex:ex + 1].to_broadcast([128, 16, 128]), op=ALU.mult),
                            reads=[r_L, r_const] + r_h2f, writes=r_h2f)
                        for q in range(4):
                            b = 4 + q
                            P.op("pe", lambda e, q=q, b=b: e.matmul(psum[b][:, :], lhsT=ones_f[:],
                                                                    rhs=diag[:, q * 512:(q + 1) * 512],
                                                                    start=True, stop=True),
                                 reads=r_h2f + [r_ones], writes=bres(b))
                            P.op("act", lambda e, q=q, b=b: e.activation(out=comb_b[:, q * 512:(q + 1) * 512],
                                                                         in_=psum[b][:, :], func=AF.Copy),
                                 reads=bres(b), writes=[r_combb])
                    else:
                        W1, W3, W2 = f_w1, f_w3, f_w2
                    for (f0, fw) in chunks:
                        nf = fw // 128
                        p1, r1 = load_panel(W1[:, f0:f0 + fw], KT, fw)
                        p3, r3 = load_panel(W3[:, f0:f0 + fw], KT, fw)
                        p2, r2 = load_panel(W2[f0:f0 + fw, :], nf, D)
                        for (t0, n) in tiles:
                            c0, c1 = t0 // 128, (t0 + n) // 128
                            hres = lambda k: [r_h2[k][c] for c in range(c0, c1)]
                            for fi in range(nf):
                                hs = hslot[0] % 4
                                hslot[0] += 1
                                asl = hs + 4 * ((hslot[0] // 4) % 2)
                                bh = 4 + 2 * (hs % 2)
                                bh3 = bh + 1
                                ph1 = psum[bh][:, 0:n]
                                ph3 = psum[bh3][:, 0:n]
                                for k in range(KT):
                                    P.op("pe", lambda e, k=k, fi=fi, ph1=ph1: e.matmul(
                                        ph1, lhsT=p1[:, k, fi * 128:(fi + 1) * 128], rhs=h2[:, k, t0:t0 + n],
                                        start=(k == 0), stop=(k == KT - 1)),
                                        reads=[r1] + hres(k), writes=bres(bh), sig=(k == KT - 1))
                                for k in range(KT):
                                    P.op("pe", lambda e, k=k, fi=fi, ph3=ph3: e.matmul(
                                        ph3, lhsT=p3[:, k, fi * 128:(fi + 1) * 128], rhs=h2[:, k, t0:t0 + n],
                                        start=(k == 0), stop=(k == KT - 1)),
                                        reads=[r3] + hres(k), writes=bres(bh3), sig=(k == KT - 1))
                                if DBG[0] == 25:
                                    continue
                                P.op("act", lambda e, hs=hs, ph1=ph1: e.activation(out=ssl[:, hs, 0:n], in_=ph1,
                                                                                   func=AF.Silu),
                                     reads=bres(bh), writes=[r_ssl[hs]])
                                if moe:
                                    P.op("dve", lambda e, hs=hs: e.tensor_tensor(
                                        out=ssl[:, hs, 0:n], in0=ssl[:, hs, 0:n],
                                        in1=comb_b[:, t0 - 128:t0 - 128 + n], op=ALU.mult),
                                        reads=[r_ssl[hs], r_combb], writes=[r_ssl[hs]])
                                P.op("dve", lambda e, hs=hs, asl=asl, ph3=ph3: e.tensor_tensor(
                                    out=act[:, asl, 0:n], in0=ph3, in1=ssl[:, hs, 0:n], op=ALU.mult),
                                    reads=bres(bh3) + [r_ssl[hs]], writes=[r_act[asl]])
                            if DBG[0] in (25, 26):
                                continue
                            flush_one()
                            asl0 = 4 * (((hslot[0] - 1) // 4) % 2)
                            hs0 = (hslot[0] - nf) % 4

                            def w2_block(n=n, t0=t0, nf=nf, p2=p2, r2=r2, hs0=hs0, hbase=hslot[0] - nf):
                                for bo in range(4):
                                    for ho in range(2):
                                        dtile = 2 * bo + ho
                                        for fi in range(nf):
                                            hs = (hbase + fi) % 4
                                            asl = hs + 4 * (((hbase + fi + 1) // 4) % 2)
                                            P.op("pe", lambda e, fi=fi, asl=asl, dtile=dtile, bo=bo, ho=ho: e.matmul(
                                                psum[bo][:, ho * 256:ho * 256 + n],
                                                lhsT=p2[:, fi, dtile * 128:(dtile + 1) * 128], rhs=act[:, asl, 0:n],
                                                start=(fi == 0), stop=(fi == nf - 1)),
                                                reads=[r2, r_act[asl]], writes=bres(bo),
                                                sig=(fi == nf - 1 and ho == 1))
                                    if DBG[0] == 27:
                                        continue
                                    P.op("dve", lambda e, bo=bo: e.tensor_tensor(
                                        out=x_all[:, 2 * bo:2 * bo + 2, t0:t0 + n],
                                        in0=psum[bo][:, :].rearrange("p (a b) -> p a b", a=2)[:, :, 0:n],
                                        in1=x_all[:, 2 * bo:2 * bo + 2, t0:t0 + n], op=ALU.add),
                                        reads=bres(bo) + xres(2 * bo, t0, n) + xres(2 * bo + 1, t0, n),
                                        writes=xres(2 * bo, t0, n) + xres(2 * bo + 1, t0, n))
                            pending.append(w2_block)
                while pending:
                    flush_one()
                P.barrier()

        def final():
            with ExitStack() as ms:
                sq = ms.enter_context(nc.sbuf_tensor("osq", [128, 4, 512], BF16))
                stat = ms.enter_context(nc.sbuf_tensor("ostat", [128, 2, 512], F32))
                yo = ms.enter_context(nc.sbuf_tensor("yo", [128, 4, KT, 512], F32))
                r_sq = [Res(f"osq{k}") for k in range(4)]
                r_stat = [Res(f"ostat{k}") for k in range(2)]
                r_yo = [[Res(f"yo{s}_{k}") for k in range(KT)] for s in range(4)]
                toks = []
                for i in range(4):
                    t0 = 128 + i * 512
                    s = i

                    def outfn(k, s=s):
                        return yo[:, s, k, :]
                    rms_tile(lambda k: xs(k, t0, 512), lambda k: xres(k, t0, 512), 512,
                             lambda k: vecs[:, 0, V_GFIN, k:k + 1], outfn, r_yo[s], sq, r_sq, stat, r_stat, alt_rstd=True)
                    for k in range(KT):
                        toks.append(P.dma("sp", "os", yT[k * 128:(k + 1) * 128, i * 512:(i + 1) * 512],
                                          yo[:, s, k, :], reads=[r_yo[s][k]]))
                P.wait_all("sp", [("os", P.dma_count["os"])])

        for l in range(depth_run):
            if DBG[0] < 20 or DBG[0] >= 30:
                mixer_layer(l)
            if DBG[0] >= 11:
                ffn_layer(l, moe=(l % 2 == 1))
        final()
        stuck, val = P.check_deadlock()
        print("deadlock check:", stuck if stuck else "OK", {n: len(v) for n, v in P.trace.items()}, val)
    return nc


def _vec8(v):
    return np.ascontiguousarray(np.asarray(v, np.float32).reshape(KT, 128).T)


_NC_CACHE = {}


def kernel(x, g_mix, w_in, conv_w, conv_b, conv_ln_g, conv_ln_b, w_conv_out,
           sg_ln_g, sg_ln_b, sg_w, sg_b, w_sg_out, w_o, g_ffn,
           ffn_w1, ffn_w3, ffn_w2, moe_router, moe_w1, moe_w3, moe_w2, g_final, _depth_run=2):
    f32 = np.float32
    x = np.asarray(x, f32)
    vecs = np.zeros((128, 2, NV, KT), f32)
    for l in range(2):
        vecs[:, l, V_GMIX] = _vec8(g_mix[l])
        vecs[:, l, V_CONVB] = _vec8(conv_b[l])
        vecs[:, l, V_CLNG] = _vec8(conv_ln_g[l])
        vecs[:, l, V_CLNB] = _vec8(conv_ln_b[l])
        vecs[:, l, V_SLNG] = _vec8(sg_ln_g[l])
        vecs[:, l, V_SLNB] = _vec8(sg_ln_b[l])
        vecs[:, l, V_GFFN] = _vec8(g_ffn[l])
        vecs[:, l, V_GFIN] = _vec8(g_final)
    cw = np.asarray(conv_w, f32)
    convw = np.ascontiguousarray(cw.transpose(2, 0, 1).reshape(KT, 128, 2, CONV_K).transpose(1, 2, 0, 3))
    sgwT = np.ascontiguousarray(np.asarray(sg_w, f32).transpose(3, 0, 1, 2))
    sgb = np.ascontiguousarray(np.broadcast_to(np.asarray(sg_b, f32)[None], (128, 2, 8, 128)))
    router = np.ascontiguousarray(np.asarray(moe_router, f32)[0].reshape(KT, 128, NE).transpose(1, 0, 2))
    tril = np.triu(np.ones((128, 128), f32))
    ident = np.eye(128, dtype=f32)
    shared = {
        "w_in": np.asarray(w_in, f32), "w_conv_out": np.asarray(w_conv_out, f32),
        "w_sg_out": np.asarray(w_sg_out, f32), "w_o": np.asarray(w_o, f32),
        "ffn_w1": np.asarray(ffn_w1, f32)[0], "ffn_w3": np.asarray(ffn_w3, f32)[0],
        "ffn_w2": np.asarray(ffn_w2, f32)[0],
        "moe_w1": np.asarray(moe_w1, f32)[0], "moe_w3": np.asarray(moe_w3, f32)[0],
        "moe_w2": np.asarray(moe_w2, f32)[0],
        "vecs": vecs, "convw": convw, "sgwT": sgwT, "sgb": sgb, "router": router,
        "tril": tril, "ident": ident,
    }
    if _depth_run < 2:
        for k in ("moe_w1", "moe_w3", "moe_w2"):
            shared.pop(k)
    in_maps = []
    for c in range(NCORES):
        b, hf = c // 2, c % 2
        start = hf * TOK
        xe = np.zeros((2304, D), f32)
        lo = start - 256
        if lo >= 0:
            xe[:] = x[b, lo:start + TOK]
        else:
            xe[256:] = x[b, 0:TOK]
        m = dict(shared)
        m["xT"] = np.ascontiguousarray(xe.T)
        m["cmask"] = np.full((128, 1), 1.0 if hf == 1 else 0.0, f32)
        in_maps.append(m)
    key = (_depth_run, DBG[0], DUMP[0])
    if key not in _NC_CACHE:
        _NC_CACHE[key] = build_nc(depth_run=_depth_run)
    nc = _NC_CACHE[key]
    res = run_bass_kernel_spmd(nc, in_maps, core_ids=list(range(NCORES)))
    LASTRES[0] = res
    out = np.empty((BATCH, SEQ, D), f32)
    for c in range(NCORES):
        b, hf = c // 2, c % 2
        out[b, hf * TOK:(hf + 1) * TOK, :] = res.results[c]["yT"].T
    return out
```

```python
import numpy as np
import concourse.bass as bass
import concourse.mybir as mybir
from concourse.bass_utils import run_bass_kernel_spmd

F32 = mybir.dt.float32
BF16 = mybir.dt.bfloat16
AF = mybir.ActivationFunctionType
ALU = mybir.AluOpType
AX = mybir.AxisListType

D = 1024
KT = 8
SEQ = 4096
BATCH = 4
NCORES = 8
TOK = 2048
NTOK = TOK + 128
CONV_K = 31
FF_DENSE = 2816
FF_EXPERT = 3584
NE = 8
EPS = 1e-6
NSLOT = 6
V_GMIX, V_CONVB, V_CLNG, V_CLNB, V_SLNG, V_SLNB, V_GFFN, V_GFIN = range(8)
NV = 8


class Res:
    __slots__ = ("name", "w", "r")

    def __init__(self, name):
        self.name = name
        self.w = None
        self.r = {}


class Eng:
    def __init__(self, name, handle, sem):
        self.name = name
        self.h = handle
        self.sem = sem
        self.count = 0
        self.known = {}
        self.pending = False


class Prog:
    def __init__(self, nc, sems):
        self.nc = nc
        self.sems = sems
        self.eng = {}
        self.dma_count = {}
        self.trace = {}

    def add_engine(self, name, handle, semname):
        self.eng[name] = Eng(name, handle, semname)

    def _wait(self, e, toks):
        need = {}
        for t in toks:
            if t is None:
                continue
            k, v = t[0], t[1]
            if k == e.sem and (len(t) < 3 or e.name == "pe"):
                continue
            if e.known.get(k, 0) >= v:
                continue
            if need.get(k, 0) < v:
                need[k] = v
        for k, v in need.items():
            e.h.wait_ge(self.sems[k], v)
            e.known[k] = v
            self.trace.setdefault(e.name, []).append(("wait", k, v))

    def _deps(self, reads, writes):
        toks = []
        for r in reads:
            if r.w is not None:
                toks.append((r.w[0], r.w[1], "raw"))
        for w in writes:
            toks.append(w.w)
            for k, v in w.r.items():
                toks.append((k, v))
        return toks

    def _commit(self, tok, reads, writes):
        for r in reads:
            k, v = tok
            if r.r.get(k, 0) < v:
                r.r[k] = v
        for w in writes:
            w.w = tok
            w.r = {}

    def op(self, ename, fn, reads=(), writes=(), sig=True):
        e = self.eng[ename]
        self._wait(e, self._deps(reads, writes))
        ins = fn(e.h)
        if sig:
            e.count += 1
            ins.then_inc(self.sems[e.sem], 1)
            self.trace.setdefault(e.name, []).append(("inc", e.sem, 1))
            e.pending = False
            tok = (e.sem, e.count)
        else:
            e.pending = True
            tok = (e.sem, e.count + 1)
        self._commit(tok, reads, writes)
        return ins

    def dma(self, qname, semname, out, in_, reads=(), writes=()):
        e = self.eng[qname]
        self._wait(e, self._deps(reads, writes))
        e.h.dma_start(out=out, in_=in_).then_inc(self.sems[semname], 16)
        self.trace.setdefault(e.name, []).append(("inc", semname, 16))
        c = self.dma_count.get(semname, 0) + 16
        self.dma_count[semname] = c
        tok = (semname, c)
        self._commit(tok, reads, writes)
        return tok

    def dma_multi(self, qname, semname, pairs, reads=(), writes=()):
        e = self.eng[qname]
        self._wait(e, self._deps(reads, writes))
        for out, in_ in pairs:
            e.h.dma_start(out=out, in_=in_).then_inc(self.sems[semname], 16)
            self.trace.setdefault(e.name, []).append(("inc", semname, 16))
        c = self.dma_count.get(semname, 0) + 16 * len(pairs)
        self.dma_count[semname] = c
        tok = (semname, c)
        self._commit(tok, reads, writes)
        return tok

    def retoken(self, semname, ress):
        tok = (semname, self.dma_count[semname])
        for r in ress:
            r.w = tok

    def barrier(self, names=("pe", "act", "dve")):
        for n in names:
            assert not self.eng[n].pending, n
        for n in names:
            e = self.eng[n]
            toks = [(self.eng[m].sem, self.eng[m].count) for m in names if m != n]
            self._wait(e, toks)

    def check_deadlock(self):
        pos = {n: 0 for n in self.trace}
        val = {}
        progress = True
        while progress:
            progress = False
            for n, ev in self.trace.items():
                while pos[n] < len(ev):
                    kind, k, v = ev[pos[n]]
                    if kind == "wait":
                        if val.get(k, 0) < v:
                            break
                    else:
                        val[k] = val.get(k, 0) + v
                    pos[n] += 1
                    progress = True
        stuck = {n: (pos[n], len(ev), ev[pos[n]]) for n, ev in self.trace.items() if pos[n] < len(ev)}
        return stuck, val

    def wait_all(self, ename, toks):
        self._wait(self.eng[ename], toks)


DBG = [99]
DUMP = [0]
LASTRES = [None]


def build_nc(depth_run=2, debug_out=None):
    nc = bass.Bass("TRN2", target_bir_lowering=False)
    dt = nc.dram_tensor
    xT = dt("xT", [D, 2304], F32, kind="ExternalInput").ap()
    w_in = dt("w_in", [2, D, 6144], F32, kind="ExternalInput").ap()
    w_co = dt("w_conv_out", [2, D, D], F32, kind="ExternalInput").ap()
    w_so = dt("w_sg_out", [2, D, D], F32, kind="ExternalInput").ap()
    w_o = dt("w_o", [2, D, D], F32, kind="ExternalInput").ap()
    f_w1 = dt("ffn_w1", [D, FF_DENSE], F32, kind="ExternalInput").ap()
    f_w3 = dt("ffn_w3", [D, FF_DENSE], F32, kind="ExternalInput").ap()
    f_w2 = dt("ffn_w2", [FF_DENSE, D], F32, kind="ExternalInput").ap()
    if depth_run >= 2:
        m_w1 = dt("moe_w1", [NE, D, FF_EXPERT], F32, kind="ExternalInput").ap()
        m_w3 = dt("moe_w3", [NE, D, FF_EXPERT], F32, kind="ExternalInput").ap()
        m_w2 = dt("moe_w2", [NE, FF_EXPERT, D], F32, kind="ExternalInput").ap()
    vecs_d = dt("vecs", [128, 2, NV, KT], F32, kind="ExternalInput").ap()
    convw_d = dt("convw", [128, 2, KT, CONV_K], F32, kind="ExternalInput").ap()
    sgwT_d = dt("sgwT", [128, 2, 8, 128], F32, kind="ExternalInput").ap()
    sgb_d = dt("sgb", [128, 2, 8, 128], F32, kind="ExternalInput").ap()
    router_d = dt("router", [128, KT, NE], F32, kind="ExternalInput").ap()
    tril_d = dt("tril", [128, 128], F32, kind="ExternalInput").ap()
    ident_d = dt("ident", [128, 128], F32, kind="ExternalInput").ap()
    cmask_d = dt("cmask", [128, 1], F32, kind="ExternalInput").ap()
    yT = dt("yT", [D, TOK], F32, kind="ExternalOutput").ap()

    from contextlib import ExitStack
    with ExitStack() as es:
        def sb(name, shape, dtype):
            return es.enter_context(nc.sbuf_tensor(name, shape, dtype))

        semnames = ["pe", "act", "dve", "cs", "xs", "os", "x0s", "sg0", "sg1"] + [f"xb{k}" for k in range(KT)] + [f"ws{i}" for i in range(NSLOT)]
        sems = {n: es.enter_context(nc.semaphore(n)) for n in semnames}
        P = Prog(nc, sems)
        P.add_engine("pe", nc.tensor, "pe")
        P.add_engine("act", nc.scalar, "act")
        P.add_engine("dve", nc.vector, "dve")
        P.add_engine("sp", nc.sync, None)
        P.add_engine("pool", nc.gpsimd, None)

        x_all = sb("x_all", [128, KT, NTOK], F32)
        ring = sb("ring", [128, NSLOT, 4096], BF16)
        vecs = sb("vecs_sb", [128, 2, NV, KT], F32)
        convw = sb("convw_sb", [128, 2, KT, CONV_K], F32)
        sgw_f = sb("sgw_f", [128, 8, 128], F32)
        sgb = sb("sgb_sb", [128, 8, 128], F32)
        router = sb("router_sb", [128, KT, NE], F32)
        tril = sb("tril_sb", [128, 128], F32)
        ident = sb("ident_sb", [128, 128], F32)
        cmask = sb("cmask_sb", [128, 1], F32)
        ones_bf = sb("ones_bf", [128, 128], BF16)
        ones_f = sb("ones_f", [128, 128], F32)
        onecol = sb("onecol", [128, 1], F32)
        epscol = sb("epscol", [128, 1], F32)
        ident_bf = sb("ident_bf", [128, 128], BF16)
        sgwT = sb("sgwT_bf", [128, 8, 128], BF16)
        Cg = sb("Cg", [128, 8, 128], F32)

        psum = [es.enter_context(nc.psum_tensor(f"ps{i}", [128, 512], F32)) for i in range(8)]
        ps_res = [Res(f"ps{i}") for i in range(8)]
        ps_half = [[Res(f"ps{i}h{h}") for h in range(2)] for i in range(8)]
        bank_ctr = [0]

        def bres(b):
            return [ps_half[b][0], ps_half[b][1]]

        def next_bank(excl=()):
            while True:
                b = bank_ctr[0] % 8
                bank_ctr[0] += 1
                if b not in excl:
                    return b

        r_x = [[Res(f"x{k}_{c}") for c in range(17)] for k in range(KT)]
        r_const = Res("const")
        r_sgwT = Res("sgwT")
        r_Cg = Res("Cg")
        slot_res = [Res(f"slot{i}") for i in range(NSLOT)]
        panel_ctr = [0]

        r_sgin = Res("sgin")
        cl = [(vecs, vecs_d), (convw, convw_d), (router, router_d),
              (tril, tril_d), (ident, ident_d), (cmask, cmask_d)]
        for t, d_ in cl:
            P.dma("sp", "cs", t[:], d_, writes=[r_const])
        P.retoken("cs", [r_const])
        def load_x():
            P.dma("sp", "xs", x_all[:, :, 0:128], xT[:, 128:256].rearrange("(k p) c -> p k c", p=128),
                  writes=[r_x[k][0] for k in range(KT)])
            for k in range(KT):
                P.dma("sp", f"xb{k}", x_all[:, k, 128:NTOK], xT[k * 128:(k + 1) * 128, 256:2304],
                      writes=[r_x[k][c] for c in range(1, 17)])
        r_ones = Res("ones")
        P.op("dve", lambda e: e.memset(ones_bf[:], 1.0), writes=[r_ones])
        P.op("dve", lambda e: e.memset(ones_f[:], 1.0), writes=[r_ones])
        P.op("dve", lambda e: e.memset(onecol[:], 1.0), writes=[r_ones])
        P.op("dve", lambda e: e.memset(epscol[:], EPS), writes=[r_ones])
        P.op("dve", lambda e: e.tensor_copy(out=ident_bf[:], in_=ident[:]), reads=[r_const], writes=[r_ones])

        dumps = []

        def dump(name, ap, shape, dtype, reads):
            if not DUMP[0]:
                return
            d_ = nc.dram_tensor("dbg_" + name, list(shape), dtype, kind="ExternalOutput").ap()
            P.dma("sp", "os", d_, ap, reads=reads)
            dumps.append(name)

        def xs(k, t0, n):
            return x_all[:, k, t0:t0 + n]

        def xres(k, t0, n):
            return [r_x[k][c] for c in range(t0 // 128, (t0 + n) // 128)]

        def load_panel(src2d, kt, cols):
            i = panel_ctr[0] % NSLOT
            panel_ctr[0] += 1
            dst = ring[:, i, 0:kt * cols].rearrange("p (k n) -> p k n", k=kt)
            srcv = src2d.rearrange("(k p) n -> p k n", p=128)
            pairs = []
            for c0 in range(0, cols, 512):
                c1 = min(cols, c0 + 512)
                pairs.append((dst[:, :, c0:c1], srcv[:, :, c0:c1]))
            P.dma_multi("pool", f"ws{i}", pairs, writes=[slot_res[i]])
            return dst, slot_res[i]

        rms_ctr = [0]

        def rms_tile(src_fn, src_res_fn, n, gcol_fn, out_fn, out_res, sq, r_sq, stat, r_stat, extra_f32=None,
                     alt_rstd=False):
            b = next_bank()
            for k in range(KT):
                s = k % 2
                P.op("act", lambda e, k=k, s=s: e.activation(out=sq[:, s, 0:n], in_=src_fn(k), func=AF.Square),
                     reads=src_res_fn(k), writes=[r_sq[s]])
                P.op("pe", lambda e, k=k, s=s: e.matmul(psum[b][:, 0:n], lhsT=ones_bf[:], rhs=sq[:, s, 0:n],
                                                        start=(k == 0), stop=(k == KT - 1)),
                     reads=[r_sq[s], r_ones], writes=bres(b), sig=True)
            rslot = rms_ctr[0] % 2 if alt_rstd else 0
            rms_ctr[0] += 1
            rstd = stat[:, rslot, 0:n]
            P.op("act", lambda e: e.activation(out=rstd, in_=psum[b][:, 0:n], func=AF.Sqrt, bias=epscol[:, 0:1],
                                               scale=1.0 / D),
                 reads=bres(b) + [r_ones], writes=[r_stat[rslot]])
            P.op("dve", lambda e: e.reciprocal(out=rstd, in_=rstd),
                 reads=[r_stat[rslot]], writes=[r_stat[rslot]])
            for k in range(KT):
                P.op("dve", lambda e, k=k: e.scalar_tensor_tensor(out=out_fn(k), in0=src_fn(k), scalar=gcol_fn(k),
                                                                  in1=rstd, op0=ALU.mult, op1=ALU.mult),
                     reads=src_res_fn(k) + [r_stat[rslot], r_const], writes=[out_res[k]])
                if extra_f32 is not None:
                    ef, er = extra_f32
                    P.op("dve", lambda e, k=k: e.scalar_tensor_tensor(out=ef(k), in0=src_fn(k), scalar=gcol_fn(k),
                                                                      in1=rstd, op0=ALU.mult, op1=ALU.mult),
                         reads=src_res_fn(k) + [r_stat[rslot], r_const], writes=[er[k]])
            return rslot

        def mm_cols(bank, n, panel, pres, col0, rhs_fn, rhs_res, kt=KT, sig_last=True):
            for k in range(kt):
                P.op("pe", lambda e, k=k: e.matmul(psum[bank][:, 0:n], lhsT=panel[:, k, col0:col0 + 128],
                                                   rhs=rhs_fn(k), start=(k == 0), stop=(k == kt - 1)),
                     reads=[pres] + rhs_res(k), writes=bres(bank), sig=(sig_last and k == kt - 1))

        def mixer_layer(l):
            with ExitStack() as ms:
                def msb(name, shape, dtype):
                    return ms.enter_context(nc.sbuf_tensor(f"{name}_{l}", shape, dtype))
                h = msb("h", [128, KT, 512], BF16)
                a_ext = msb("a_ext", [128, KT, 30 + 512], BF16)
                cv = msb("cv", [128, KT, 512], F32)
                ma = msb("ma", [128, KT, 512], BF16)
                gu = msb("gu", [128, KT, 512], BF16)
                sq = msb("sq", [128, 4, 512], BF16)
                sg = msb("sg", [128, 2, 512], F32)
                stat = msb("stat", [128, 4, 512], F32)
                small = msb("small", [128, 16, 8], F32)
                dgA = msb("dgA", [128, 16, 128], BF16)
                dgB = msb("dgB", [128, 16, 128], BF16)
                r_dgA = Res("dgA")
                r_dgB = Res("dgB")
                r_h = [Res(f"h{k}") for k in range(KT)]
                r_a = [Res(f"a{k}") for k in range(KT)]
                r_cv = [Res(f"cv{k}") for k in range(KT)]
                r_ma = [Res(f"ma{k}") for k in range(KT)]
                r_gu = [Res(f"gu{k}") for k in range(KT)]
                r_sq = [Res(f"sq{k}") for k in range(4)]
                r_sg = [Res(f"sg{k}") for k in range(2)]
                r_stat = [Res(f"stat{k}") for k in range(4)]
                x0 = cv[:, 0:2, :].rearrange("p a (b c) -> p (a b) c", c=128)
                r_x0 = [r_cv[0]] * 4 + [r_cv[1]] * 4
                r_small = Res("small")
                sg_ctr = [0]
                gvs = [cv[:, 0:2, :].rearrange("p a b -> p (a b)"),
                       cv[:, 2:4, :].rearrange("p a b -> p (a b)")]
                gv = gvs[0]
                sqv = dgA[:, :, :].rearrange("p a b -> p (a b)").bitcast(F32)
                vn = cv[:, 4:8, :].rearrange("p a b -> p (a b)").bitcast(BF16).rearrange("p (c n) -> p c n", c=4)
                r_gvs = [[r_cv[0], r_cv[1]], [r_cv[2], r_cv[3]]]
                r_gv = r_gvs[0]
                r_sqv = [r_dgA]

                def vcol(idx, j):
                    return vecs[:, l, idx, j:j + 1]

                P.dma_multi("sp", f"sg{l}", [(sgw_f[:], sgwT_d[:, l, :, :]), (sgb[:], sgb_d[:, l, :, :])],
                            writes=[r_sgin])
                P.op("dve", lambda e: e.tensor_tensor(out=sgwT[:], in0=sgw_f[:, :, :],
                                                      in1=tril[:].unsqueeze(1).to_broadcast([128, 8, 128]),
                                                      op=ALU.mult),
                     reads=[r_const, r_sgin], writes=[r_sgwT])
                for half in range(2):
                    b = next_bank()
                    P.op("pe", lambda e, half=half, b=b: e.matmul(
                        psum[b][:, :], lhsT=ones_bf[:],
                        rhs=sgwT[:, half * 4:(half + 1) * 4, :].rearrange("p g t -> p (g t)"),
                        start=True, stop=True), reads=[r_sgwT, r_ones], writes=bres(b))
                    for gg in range(4):
                        g = half * 4 + gg
                        P.op("dve", lambda e, g=g, gg=gg, b=b: e.scalar_tensor_tensor(
                            out=Cg[:, g, :], in0=psum[b][:, gg * 128:(gg + 1) * 128], scalar=vcol(V_SLNB, g),
                            in1=sgb[:, g, :], op0=ALU.mult, op1=ALU.add),
                            reads=bres(b) + [r_const, r_sgin], writes=[r_Cg])

                if l == 0:
                    load_x()

                h_ready = [None]

                def m1(t0, n, use_x0=False):
                    if use_x0:
                        src_fn = lambda k: x0[:, k, 0:n]
                        src_res = lambda k: [r_x0[k]]
                    else:
                        src_fn = lambda k: xs(k, t0, n)
                        src_res = lambda k: xres(k, t0, n)
                    rms_tile(src_fn, src_res, n, lambda k: vcol(V_GMIX, k), lambda k: h[:, k, 0:n], r_h,
                             sq, r_sq, stat, r_stat)
                    h_ready[0] = (t0, n, use_x0)

                def tile(t0, n, a_only, use_x0=False, masked=False, nxt=None):
                    nch = n // 128
                    if h_ready[0] != (t0, n, use_x0):
                        m1(t0, n, use_x0)
                    h_ready[0] = None
                    hk = lambda k: h[:, k, 0:n]
                    hres = lambda k: [r_h[k]]
                    W = w_in[l]
                    mcol = cmask[:, 0:1] if masked else onecol[:, 0:1]

                    def build_diag(j):
                        P.op("dve", lambda e: e.tensor_tensor(
                            out=dgA[:, 0:16, :], in0=ident_bf[:].unsqueeze(1).to_broadcast([128, 16, 128]),
                            in1=convw[:, l, j, 0:16].unsqueeze(2).to_broadcast([128, 16, 128]), op=ALU.mult),
                            reads=[r_const, r_ones], writes=[r_dgA])
                        P.op("dve", lambda e: e.tensor_tensor(
                            out=dgB[:, 0:15, :], in0=ident_bf[:].unsqueeze(1).to_broadcast([128, 15, 128]),
                            in1=convw[:, l, j, 16:31].unsqueeze(2).to_broadcast([128, 15, 128]), op=ALU.mult),
                            reads=[r_const, r_ones], writes=[r_dgB])
                    if not a_only:
                        build_diag(0)
                    for jj in range(2):
                        pv, rv = load_panel(W[:, jj * 512:(jj + 1) * 512], KT, 512)
                        pg, rg = load_panel(W[:, 1024 + jj * 512:1024 + (jj + 1) * 512], KT, 512)
                        for j4 in range(4):
                            j = jj * 4 + j4
                            b1 = next_bank()
                            b2 = next_bank()
                            mm_cols(b1, n, pv, rv, j4 * 128, hk, hres)
                            mm_cols(b2, n, pg, rg, j4 * 128, hk, hres)
                            s = sg_ctr[0] % 2
                            sg_ctr[0] += 1
                            P.op("act", lambda e, s=s, b2=b2: e.activation(out=sg[:, s, 0:n], in_=psum[b2][:, 0:n],
                                                                          func=AF.Sigmoid),
                                 reads=bres(b2), writes=[r_sg[s]])
                            P.op("dve", lambda e, s=s, b1=b1, j=j: e.scalar_tensor_tensor(
                                out=a_ext[:, j, 30:30 + n], in0=psum[b1][:, 0:n], scalar=mcol, in1=sg[:, s, 0:n],
                                op0=ALU.mult, op1=ALU.mult),
                                reads=bres(b1) + [r_sg[s], r_const, r_ones], writes=[r_a[j]])
                    if a_only:
                        for j in range(KT):
                            P.op("act", lambda e, j=j: e.activation(out=a_ext[:, j, 0:30], in_=a_ext[:, j, n:n + 30],
                                                                    func=AF.Copy),
                                 reads=[r_a[j]], writes=[r_a[j]])
                        return
                    if DBG[0] <= 2:
                        return
                    bmu, bsq = 6, 7
                    pu = ru = None
                    stat_pending = []
                    for j in range(KT):
                        b = next_bank(excl=(bmu, bsq))
                        if j > 0:
                            build_diag(j)
                        for k in range(CONV_K):
                            dgt, rdg, kk = (dgA, r_dgA, k) if k < 16 else (dgB, r_dgB, k - 16)
                            P.op("pe", lambda e, j=j, k=k, kk=kk, dgt=dgt, b=b: e.matmul(
                                psum[b][:, 0:n], lhsT=dgt[:, kk, :], rhs=a_ext[:, j, k:k + n],
                                start=(k == 0), stop=(k == CONV_K - 1)),
                                reads=[rdg, r_a[j]], writes=bres(b), sig=(k == 15 or k == CONV_K - 1))
                        while stat_pending:
                            stat_pending.pop(0)()
                        s0, s1 = (2 * j) % 4, (2 * j + 1) % 4
                        P.op("act", lambda e, j=j, b=b: e.activation(out=cv[:, j, 0:n], in_=psum[b][:, 0:n],
                                                                      func=AF.Identity, bias=vcol(V_CONVB, j),
                                                                      scale=1.0),
                             reads=bres(b) + [r_const], writes=[r_cv[j]])
                        P.op("act", lambda e, j=j, b=b, s0=s0: e.activation(out=sq[:, s0, 0:n], in_=psum[b][:, 0:n],
                                                                            func=AF.Identity,
                                                                            bias=vcol(V_CONVB, j), scale=1.0),
                             reads=bres(b) + [r_const], writes=[r_sq[s0]])
                        P.op("act", lambda e, j=j, b=b, s1=s1: e.activation(out=sq[:, s1, 0:n], in_=psum[b][:, 0:n],
                                                                            func=AF.Square,
                                                                            bias=vcol(V_CONVB, j), scale=1.0),
                             reads=bres(b) + [r_const], writes=[r_sq[s1]])
                        def stat_mm(j=j, s0=s0, s1=s1):
                            P.op("pe", lambda e: e.matmul(psum[bmu][:, 0:n], lhsT=ones_bf[:],
                                                          rhs=sq[:, s0, 0:n], start=(j == 0), stop=(j == KT - 1)),
                                 reads=[r_sq[s0], r_ones], writes=bres(bmu))
                            P.op("pe", lambda e: e.matmul(psum[bsq][:, 0:n], lhsT=ones_bf[:],
                                                          rhs=sq[:, s1, 0:n], start=(j == 0), stop=(j == KT - 1)),
                                 reads=[r_sq[s1], r_ones], writes=bres(bsq))
                        stat_pending.append(stat_mm)
                        P.op("act", lambda e, j=j: e.activation(out=a_ext[:, j, 0:30], in_=a_ext[:, j, n:n + 30],
                                                                func=AF.Copy),
                             reads=[r_a[j]], writes=[r_a[j]])
                    if DBG[0] <= 3:
                        return
                    mean = stat[:, 1, 0:n]
                    var = stat[:, 2, 0:n]

                    def ln_head():
                        P.op("dve", lambda e: e.tensor_scalar(out=mean, in0=psum[bmu][:, 0:n], scalar1=1.0 / D,
                                                              scalar2=None, op0=ALU.mult),
                             reads=bres(bmu), writes=[r_stat[1]])
                        P.op("dve", lambda e: e.tensor_tensor(out=var, in0=mean, in1=mean, op=ALU.mult),
                             reads=[r_stat[1]], writes=[r_stat[2]])
                        P.op("dve", lambda e: e.scalar_tensor_tensor(out=var, in0=psum[bsq][:, 0:n], scalar=1.0 / D,
                                                                     in1=var, op0=ALU.mult, op1=ALU.subtract),
                             reads=bres(bsq) + [r_stat[2]], writes=[r_stat[2]])
                        for j in range(KT):
                            P.op("dve", lambda e, j=j: e.tensor_tensor(out=cv[:, j, 0:n], in0=cv[:, j, 0:n],
                                                                       in1=mean, op=ALU.subtract),
                                 reads=[r_cv[j], r_stat[1]], writes=[r_cv[j]])

                    upend = stat_pending
                    for j in range(KT):
                        if j % 4 == 0:
                            pu, ru = load_panel(W[:, 2048 + (j // 4) * 512:2048 + (j // 4 + 1) * 512], KT, 512)
                        bu = next_bank(excl=(bmu, bsq))
                        mm_cols(bu, n, pu, ru, (j % 4) * 128, hk, hres)
                        if j == 0:
                            while upend:
                                upend.pop(0)()
                            ln_head()
                        P.op("act", lambda e, bu=bu, j=j: e.activation(out=gu[:, j, 0:n], in_=psum[bu][:, 0:n],
                                                                      func=AF.Gelu),
                             reads=bres(bu), writes=[r_gu[j]])
                        if j == 0:
                            P.op("act", lambda e: e.activation(out=var, in_=var, func=AF.Sqrt, bias=epscol[:, 0:1],
                                                               scale=1.0),
                                 reads=[r_stat[2], r_ones], writes=[r_stat[2]])
                            P.op("dve", lambda e: e.reciprocal(out=var, in_=var),
                                 reads=[r_stat[2]], writes=[r_stat[2]])
                            for jn in range(KT):
                                P.op("dve", lambda e, jn=jn: e.tensor_tensor(out=cv[:, jn, 0:n], in0=cv[:, jn, 0:n],
                                                                             in1=var, op=ALU.mult),
                                     reads=[r_cv[jn], r_stat[2]], writes=[r_cv[jn]])
                    for j in range(KT):
                        P.op("act", lambda e, j=j: e.activation(out=a_ext[:, j, 30:30 + n], in_=cv[:, j, 0:n],
                                                                func=AF.Silu, bias=vcol(V_CLNB, j),
                                                                scale=vcol(V_CLNG, j)),
                             reads=[r_cv[j], r_const], writes=[r_a[j]])
                    for jj in range(2):
                        pg, rg = load_panel(W[:, 4096 + jj * 512:4096 + (jj + 1) * 512], KT, 512)
                        for j4 in range(4):
                            j = jj * 4 + j4
                            b2 = next_bank(excl=(bmu, bsq))
                            mm_cols(b2, n, pg, rg, j4 * 128, hk, hres)
                            P.op("act", lambda e, b2=b2, j=j: e.activation(out=ma[:, j, 0:n], in_=psum[b2][:, 0:n],
                                                                          func=AF.Sigmoid),
                                 reads=bres(b2), writes=[r_ma[j]])
                    cak = lambda k: a_ext[:, k, 30:30 + n]
                    cares = lambda k: [r_a[k]]
                    if DBG[0] <= 4:
                        return
                    for jj in range(2):
                        pc, rc = load_panel(w_co[l][:, jj * 512:(jj + 1) * 512], KT, 512)
                        for j4 in range(4):
                            j = jj * 4 + j4
                            b1 = next_bank()
                            mm_cols(b1, n, pc, rc, j4 * 128, cak, cares)
                            P.op("dve", lambda e, b1=b1, j=j: e.tensor_tensor(
                                out=ma[:, j, 0:n], in0=psum[b1][:, 0:n], in1=ma[:, j, 0:n], op=ALU.mult),
                                reads=bres(b1) + [r_ma[j]], writes=[r_ma[j]])
                    if DBG[0] <= 5:
                        return
                    if t0 == 128 and l == 0:
                        dump("h", h[:], [128, KT, 512], BF16, r_h)
                        dump("ca", a_ext[:], [128, KT, 542], BF16, r_a)
                        dump("ma1", ma[:], [128, KT, 512], BF16, r_ma)
                        dump("gu", gu[:], [128, KT, 512], BF16, r_gu)
                    if DBG[0] <= 6:
                        return
                    pv0, rv0 = load_panel(W[:, 3072:3584], KT, 512)
                    pv1, rv1 = load_panel(W[:, 3584:4096], KT, 512)
                    def v_stage_a(c):
                        gvc, r_gvc, so = gvs[c % 2], r_gvs[c % 2], 8 * (c % 2)
                        for half, (pv, rv) in enumerate(((pv0, rv0), (pv1, rv1))):
                            b1 = next_bank()
                            for k in range(KT):
                                P.op("pe", lambda e, k=k, b1=b1, pv=pv: e.matmul(
                                    psum[b1][:, :], lhsT=h[:, k, c * 128:(c + 1) * 128], rhs=pv[:, k, :],
                                    start=(k == 0), stop=(k == KT - 1)),
                                    reads=[rv, r_h[k]], writes=bres(b1), sig=(k == KT - 1))
                            P.op("act", lambda e, b1=b1, half=half: e.activation(
                                out=gvc[:, half * 512:(half + 1) * 512], in_=psum[b1][:, :], func=AF.Gelu),
                                reads=bres(b1), writes=[r_gvc[half]])
                        P.op("act", lambda e: e.activation(out=sqv, in_=gvc, func=AF.Square),
                             reads=r_gvc, writes=r_sqv)
                        s1 = small[:, so + 0, :]
                        s2 = small[:, so + 1, :]
                        mu = small[:, so + 2, :]
                        rs = small[:, so + 3, :]
                        r_sm = r_smalls[c % 2]
                        P.op("dve", lambda e: e.tensor_reduce(out=s1, in_=gvc.rearrange("p (g d) -> p g d", g=8),
                                                              axis=AX.X, op=ALU.add),
                             reads=r_gvc, writes=[r_sm])
                        P.op("dve", lambda e: e.tensor_reduce(out=s2, in_=sqv.rearrange("p (g d) -> p g d", g=8),
                                                              axis=AX.X, op=ALU.add),
                             reads=r_sqv, writes=[r_sm])
                        P.op("dve", lambda e: e.tensor_scalar(out=mu, in0=s1, scalar1=1.0 / 128, scalar2=None,
                                                              op0=ALU.mult), reads=[r_sm], writes=[r_sm])
                        P.op("dve", lambda e: e.tensor_tensor(out=rs, in0=mu, in1=mu, op=ALU.mult),
                             reads=[r_sm], writes=[r_sm])
                        P.op("dve", lambda e: e.scalar_tensor_tensor(out=rs, in0=s2, scalar=1.0 / 128, in1=rs,
                                                                     op0=ALU.mult, op1=ALU.subtract),
                             reads=[r_sm], writes=[r_sm])

                    def v_stage_b(c):
                        gvc, r_gvc, so = gvs[c % 2], r_gvs[c % 2], 8 * (c % 2)
                        mu = small[:, so + 2, :]
                        rs = small[:, so + 3, :]
                        nb = small[:, so + 4, :]
                        r_sm = r_smalls[c % 2]
                        P.op("act", lambda e: e.activation(out=rs, in_=rs, func=AF.Sqrt, bias=epscol[:, 0:1],
                                                           scale=1.0),
                             reads=[r_sm, r_ones], writes=[r_sm])
                        P.op("dve", lambda e: e.reciprocal(out=rs, in_=rs),
                             reads=[r_sm], writes=[r_sm])
                        P.op("dve", lambda e: e.scalar_tensor_tensor(out=nb, in0=mu, scalar=-1.0, in1=rs,
                                                                     op0=ALU.mult, op1=ALU.mult),
                             reads=[r_sm], writes=[r_sm])
                        P.op("dve", lambda e: e.tensor_tensor(
                            out=sqv.rearrange("p (g d) -> p g d", g=8), in0=gvc.rearrange("p (g d) -> p g d", g=8),
                            in1=rs.unsqueeze(2).to_broadcast([128, 8, 128]), op=ALU.mult),
                            reads=r_gvc + [r_sm], writes=r_sqv)
                        P.op("dve", lambda e: e.tensor_tensor(
                            out=vn[:, c, :].rearrange("p (g d) -> p g d", g=8),
                            in0=sqv.rearrange("p (g d) -> p g d", g=8),
                            in1=nb.unsqueeze(2).to_broadcast([128, 8, 128]), op=ALU.add),
                            reads=r_sqv + [r_sm], writes=[r_cv[4 + c]])

                    r_smalls = [Res("small0"), Res("small1")]
                    for c in range(nch + 1):
                        if c < nch:
                            v_stage_a(c)
                        if c >= 1:
                            v_stage_b(c - 1)
                    for jj in range(2):
                        pg, rg = load_panel(W[:, 5120 + jj * 512:5120 + (jj + 1) * 512], KT, 512)
                        for j4 in range(4):
                            j = jj * 4 + j4
                            b2 = next_bank()
                            mm_cols(b2, n, pg, rg, j4 * 128, hk, hres)
                            P.op("act", lambda e, b2=b2, j=j: e.activation(out=a_ext[:, j, 30:30 + n],
                                                                          in_=psum[b2][:, 0:n], func=AF.Sigmoid),
                                 reads=bres(b2), writes=[r_a[j]])
                    if t0 == 128 and l == 0:
                        dump("vn", vn, [128, 4, 1024], BF16, r_cv[4:8])
                        dump("gv", gv, [128, 1024], F32, r_gv)
                        dump("small", small[:], [128, 8, 8], F32, [r_small])
                    if DBG[0] <= 7:
                        return
                    for g in range(8):
                        b1 = next_bank()
                        for c in range(nch):
                            P.op("pe", lambda e, g=g, c=c, b1=b1: e.matmul(
                                psum[b1][:, c * 128:(c + 1) * 128], lhsT=vn[:, c, g * 128:(g + 1) * 128],
                                rhs=sgwT[:, g, :], start=True, stop=True),
                                reads=[r_cv[4 + c], r_sgwT], writes=bres(b1), sig=(c == nch - 1))
                        tmp = stat[:, 3, 0:n]
                        P.op("dve", lambda e, g=g, b1=b1: e.scalar_tensor_tensor(
                            out=tmp.rearrange("p (c t) -> p c t", c=nch),
                            in0=psum[b1][:, 0:n].rearrange("p (c t) -> p c t", c=nch),
                            scalar=vcol(V_SLNG, g),
                            in1=Cg[:, g, :].unsqueeze(1).to_broadcast([128, nch, 128]),
                            op0=ALU.mult, op1=ALU.add),
                            reads=bres(b1) + [r_Cg, r_const], writes=[r_stat[3]])
                        P.op("dve", lambda e, g=g: e.tensor_tensor(out=gu[:, g, 0:n], in0=gu[:, g, 0:n], in1=tmp,
                                                                   op=ALU.mult),
                             reads=[r_gu[g], r_stat[3]], writes=[r_gu[g]])
                    yk = lambda k: gu[:, k, 0:n]
                    yres = lambda k: [r_gu[k]]
                    if t0 == 128 and l == 0:
                        dump("y", gu[:], [128, KT, 512], BF16, r_gu)
                        dump("Cg", Cg[:], [128, 8, 128], F32, [r_Cg])
                    if DBG[0] <= 8:
                        return
                    for jj in range(2):
                        pc, rc = load_panel(w_so[l][:, jj * 512:(jj + 1) * 512], KT, 512)
                        for j4 in range(4):
                            j = jj * 4 + j4
                            b1 = next_bank()
                            mm_cols(b1, n, pc, rc, j4 * 128, yk, yres)
                            s = sg_ctr[0] % 2
                            sg_ctr[0] += 1
                            P.op("dve", lambda e, s=s, b1=b1, j=j: e.tensor_tensor(
                                out=sg[:, s, 0:n], in0=psum[b1][:, 0:n], in1=a_ext[:, j, 30:30 + n], op=ALU.mult),
                                reads=bres(b1) + [r_a[j]], writes=[r_sg[s]])
                            P.op("dve", lambda e, s=s, j=j: e.tensor_tensor(
                                out=ma[:, j, 0:n], in0=ma[:, j, 0:n], in1=sg[:, s, 0:n], op=ALU.add),
                                reads=[r_ma[j], r_sg[s]], writes=[r_ma[j]])
                    if t0 == 128 and l == 0:
                        dump("m", ma[:], [128, KT, 512], BF16, r_ma)
                    if DBG[0] <= 9:
                        return
                    if nxt is not None:
                        m1(nxt[0], nxt[1])
                    mk = lambda k: ma[:, k, 0:n]
                    mres = lambda k: [r_ma[k]]
                    for jj in range(2):
                        po, ro = load_panel(w_o[l][:, jj * 512:(jj + 1) * 512], KT, 512)
                        for j4 in range(4):
                            j = jj * 4 + j4
                            b1 = next_bank()
                            mm_cols(b1, n, po, ro, j4 * 128, mk, mres)
                            P.op("dve", lambda e, b1=b1, j=j: e.tensor_tensor(
                                out=xs(j, t0, n), in0=psum[b1][:, 0:n], in1=xs(j, t0, n), op=ALU.add),
                                reads=bres(b1) + xres(j, t0, n), writes=xres(j, t0, n))

                if l == 0:
                    for j in range(KT):
                        P.op("dve", lambda e, j=j: e.memset(a_ext[:, j, 0:30], 0.0), writes=[r_a[j]])
                    tile(0, 128, False, nxt=(128, 512))
                else:
                    tile(0, 128, True, masked=True)
                for i in range(4):
                    tile(128 + i * 512, 512, False, nxt=((128 + (i + 1) * 512, 512) if i < 3 else None))
                P.barrier()

        def ffn_layer(l, moe):
            with ExitStack() as ms:
                def msb(name, shape, dtype):
                    return ms.enter_context(nc.sbuf_tensor(f"f{name}_{l}", shape, dtype))
                h2 = msb("h2", [128, KT, NTOK], BF16)
                sq = msb("sq", [128, 4, 256], BF16)
                stat = msb("stat", [128, 2, 256], F32)
                ssl = msb("ssl", [128, 4, 256], F32)
                act = msb("act", [128, 8, 256], BF16)
                r_h2 = [[Res(f"h2_{k}_{c}") for c in range(17)] for k in range(KT)]
                r_sq = [Res(f"fsq{k}") for k in range(4)]
                r_stat = [Res(f"fstat{k}") for k in range(2)]
                r_ssl = [Res(f"ssl{k}") for k in range(4)]
                r_act = [Res(f"act{k}") for k in range(8)]
                if moe:
                    diagbuf = msb("diagbuf", [128, TOK], F32)
                    router_g = msb("router_g", [128, KT, NE], F32)
                    rtok = msb("rtok", [128, 16], F32)
                    r_rg = Res("router_g")
                    r_rtok = Res("rtok")
                    comb_b = msb("comb_b", [128, TOK], F32)
                    L = msb("L", [128, 16, NE], F32)
                    L2 = msb("L2", [128, 16, NE], F32)
                    comb = msb("comb", [128, 16, NE], F32)
                    m12 = msb("m12", [128, 4, 16], F32)
                    r_h2f = [Res("diagbuf")]
                    r_combb = Res("comb_b")
                    r_L = Res("L")
                    diag = diagbuf[:, :]
                    P.op("dve", lambda e: e.tensor_tensor(
                        out=router_g[:], in0=router[:],
                        in1=vecs[:, l, V_GFFN, :].unsqueeze(2).to_broadcast([128, KT, NE]), op=ALU.mult),
                        reads=[r_const], writes=[r_rg])
                tiles = ([] if moe else [(0, 128)]) + [(128 + 256 * i, 256) for i in range(8)]

                for (t0, n) in tiles:
                    out_res = [Res("tmp") for _ in range(KT)]
                    rslot = rms_tile(lambda k: xs(k, t0, n), lambda k: xres(k, t0, n), n,
                                     lambda k: vecs[:, l, V_GFFN, k:k + 1],
                                     lambda k: h2[:, k, t0:t0 + n], out_res, sq, r_sq, stat, r_stat, alt_rstd=True)
                    for k in range(KT):
                        for c in range(t0 // 128, (t0 + n) // 128):
                            r_h2[k][c].w = out_res[k].w
                    if moe:
                        for cc in range(n // 128):
                            c = (t0 - 128) // 128 + cc
                            tc0 = t0 + cc * 128
                            b = next_bank()
                            for k in range(KT):
                                P.op("pe", lambda e, k=k, tc0=tc0, b=b: e.matmul(
                                    psum[b][:, 0:NE], lhsT=x_all[:, k, tc0:tc0 + 128], rhs=router_g[:, k, :],
                                    start=(k == 0), stop=(k == KT - 1)),
                                    reads=xres(k, tc0, 128) + [r_rg], writes=bres(b), sig=(k == KT - 1))
                            P.op("pe", lambda e, cc=cc, b=b, rslot=rslot: e.matmul(
                                psum[b][:, NE:NE + 1], lhsT=stat[:, rslot, cc * 128:(cc + 1) * 128],
                                rhs=ident[:, 0:1], start=True, stop=True),
                                reads=[r_stat[rslot], r_const], writes=bres(b))
                            P.op("act", lambda e, c=c, b=b: e.activation(out=rtok[:, c:c + 1],
                                                                         in_=psum[b][:, NE:NE + 1], func=AF.Copy),
                                 reads=bres(b), writes=[r_rtok])
                            P.op("dve", lambda e, c=c, b=b: e.tensor_scalar(
                                out=L[:, c, :], in0=psum[b][:, 0:NE], scalar1=rtok[:, c:c + 1], scalar2=None,
                                op0=ALU.mult),
                                reads=bres(b) + [r_rtok], writes=[r_L])
                if moe:
                    m1 = m12[:, 0, :]
                    m2 = m12[:, 1, :]
                    den = m12[:, 2, :]

                    def bc(a):
                        return a.unsqueeze(2).to_broadcast([128, 16, NE])
                    ops = [
                        lambda e: e.tensor_reduce(out=m1, in_=L[:], axis=AX.X, op=ALU.max),
                        lambda e: e.tensor_tensor(out=L2[:], in0=L[:], in1=bc(m1), op=ALU.is_equal),
                        lambda e: e.scalar_tensor_tensor(out=L2[:], in0=L2[:], scalar=-1e30, in1=L[:],
                                                         op0=ALU.mult, op1=ALU.add),
                        lambda e: e.tensor_reduce(out=m2, in_=L2[:], axis=AX.X, op=ALU.max),
                        lambda e: e.tensor_tensor(out=L2[:], in0=L[:], in1=bc(m2), op=ALU.is_ge),
                        lambda e: e.tensor_tensor(out=comb[:], in0=L[:], in1=bc(m1), op=ALU.subtract),
                    ]
                    for f in ops:
                        P.op("dve", f, reads=[r_L], writes=[r_L])
                    P.op("act", lambda e: e.activation(out=comb[:], in_=comb[:], func=AF.Exp),
                         reads=[r_L], writes=[r_L])
                    ops = [
                        lambda e: e.tensor_tensor(out=comb[:], in0=comb[:], in1=L2[:], op=ALU.mult),
                        lambda e: e.tensor_reduce(out=den, in_=comb[:], axis=AX.X, op=ALU.add),
                        lambda e: e.reciprocal(out=den, in_=den),
                        lambda e: e.tensor_tensor(out=comb[:], in0=comb[:], in1=bc(den), op=ALU.mult),
                    ]
                    for f in ops:
                        P.op("dve", f, reads=[r_L], writes=[r_L])

                nexp = NE if moe else 1
                FF = FF_EXPERT if moe else FF_DENSE
                chunks = []
                f0 = 0
                while f0 < FF:
                    w = min(512, FF - f0)
                    chunks.append((f0, w))
                    f0 += w
                hslot = [0]
                pending = []
                if DBG[0] == 21:
                    chunks = []
                if DBG[0] == 22:
                    chunks = chunks[:1]
                if DBG[0] == 23:
                    chunks = chunks[-1:]
                if DBG[0] in (24, 25, 26, 27):
                    chunks = chunks[:1]
                    tiles = tiles[1:2]

                def flush_one():
                    if pending:
                        pending.pop(0)()

                for ex in range(nexp):
                    if moe:
                        while pending:
                            flush_one()
                        W1, W3, W2 = m_w1[ex], m_w3[ex], m_w2[ex]
                        P.op("dve", lambda e, ex=ex: e.tensor_tensor(
                            out=diag.rearrange("p (c t) -> p c t", c=16),
                            in0=ident[:].unsqueeze(1).to_broadcast([128, 16, 128]),
                            in1=comb[:, :, ex:ex + 1].to_broadcast([128, 16, 128]), op=ALU.mult),
                            reads=[r_L, r_const] + r_h2f, writes=r_h2f)
                        for q in range(4):
                            b = 4 + q
                            P.op("pe", lambda e, q=q, b=b: e.matmul(psum[b][:, :], lhsT=ones_f[:],
                                                                    rhs=diag[:, q * 512:(q + 1) * 512],
                                                                    start=True, stop=True),
                                 reads=r_h2f + [r_ones], writes=bres(b))
                            P.op("act", lambda e, q=q, b=b: e.activation(out=comb_b[:, q * 512:(q + 1) * 512],
                                                                         in_=psum[b][:, :], func=AF.Copy),
                                 reads=bres(b), writes=[r_combb])
                    else:
                        W1, W3, W2 = f_w1, f_w3, f_w2
                    for (f0, fw) in chunks:
                        nf = fw // 128
                        p1, r1 = load_panel(W1[:, f0:f0 + fw], KT, fw)
                        p3, r3 = load_panel(W3[:, f0:f0 + fw], KT, fw)
                        p2, r2 = load_panel(W2[f0:f0 + fw, :], nf, D)
                        for (t0, n) in tiles:
                            c0, c1 = t0 // 128, (t0 + n) // 128
                            hres = lambda k: [r_h2[k][c] for c in range(c0, c1)]
                            for fi in range(nf):
                                hs = hslot[0] % 4
                                hslot[0] += 1
                                asl = hs + 4 * ((hslot[0] // 4) % 2)
                                bh = 4 + 2 * (hs % 2)
                                bh3 = bh + 1
                                ph1 = psum[bh][:, 0:n]
                                ph3 = psum[bh3][:, 0:n]
                                for k in range(KT):
                                    P.op("pe", lambda e, k=k, fi=fi, ph1=ph1: e.matmul(
                                        ph1, lhsT=p1[:, k, fi * 128:(fi + 1) * 128], rhs=h2[:, k, t0:t0 + n],
                                        start=(k == 0), stop=(k == KT - 1)),
                                        reads=[r1] + hres(k), writes=bres(bh), sig=(k == KT - 1))
                                for k in range(KT):
                                    P.op("pe", lambda e, k=k, fi=fi, ph3=ph3: e.matmul(
                                        ph3, lhsT=p3[:, k, fi * 128:(fi + 1) * 128], rhs=h2[:, k, t0:t0 + n],
                                        start=(k == 0), stop=(k == KT - 1)),
                                        reads=[r3] + hres(k), writes=bres(bh3), sig=(k == KT - 1))
                                if DBG[0] == 25:
                                    continue
                                P.op("act", lambda e, hs=hs, ph1=ph1: e.activation(out=ssl[:, hs, 0:n], in_=ph1,
                                                                                   func=AF.Silu),
                                     reads=bres(bh), writes=[r_ssl[hs]])
                                if moe:
                                    P.op("dve", lambda e, hs=hs: e.tensor_tensor(
                                        out=ssl[:, hs, 0:n], in0=ssl[:, hs, 0:n],
                                        in1=comb_b[:, t0 - 128:t0 - 128 + n], op=ALU.mult),
                                        reads=[r_ssl[hs], r_combb], writes=[r_ssl[hs]])
                                P.op("dve", lambda e, hs=hs, asl=asl, ph3=ph3: e.tensor_tensor(
                                    out=act[:, asl, 0:n], in0=ph3, in1=ssl[:, hs, 0:n], op=ALU.mult),
                                    reads=bres(bh3) + [r_ssl[hs]], writes=[r_act[asl]])
                            if DBG[0] in (25, 26):
                                continue
                            flush_one()
                            asl0 = 4 * (((hslot[0] - 1) // 4) % 2)
                            hs0 = (hslot[0] - nf) % 4

                            def w2_block(n=n, t0=t0, nf=nf, p2=p2, r2=r2, hs0=hs0, hbase=hslot[0] - nf):
                                for bo in range(4):
                                    for ho in range(2):
                                        dtile = 2 * bo + ho
                                        for fi in range(nf):
                                            hs = (hbase + fi) % 4
                                            asl = hs + 4 * (((hbase + fi + 1) // 4) % 2)
                                            P.op("pe", lambda e, fi=fi, asl=asl, dtile=dtile, bo=bo, ho=ho: e.matmul(
                                                psum[bo][:, ho * 256:ho * 256 + n],
                                                lhsT=p2[:, fi, dtile * 128:(dtile + 1) * 128], rhs=act[:, asl, 0:n],
                                                start=(fi == 0), stop=(fi == nf - 1)),
                                                reads=[r2, r_act[asl]], writes=bres(bo),
                                                sig=(fi == nf - 1 and ho == 1))
                                    if DBG[0] == 27:
                                        continue
                                    P.op("dve", lambda e, bo=bo: e.tensor_tensor(
                                        out=x_all[:, 2 * bo:2 * bo + 2, t0:t0 + n],
                                        in0=psum[bo][:, :].rearrange("p (a b) -> p a b", a=2)[:, :, 0:n],
                                        in1=x_all[:, 2 * bo:2 * bo + 2, t0:t0 + n], op=ALU.add),
                                        reads=bres(bo) + xres(2 * bo, t0, n) + xres(2 * bo + 1, t0, n),
                                        writes=xres(2 * bo, t0, n) + xres(2 * bo + 1, t0, n))
                            pending.append(w2_block)
                while pending:
                    flush_one()
                P.barrier()

        def final():
            with ExitStack() as ms:
                sq = ms.enter_context(nc.sbuf_tensor("osq", [128, 4, 512], BF16))
                stat = ms.enter_context(nc.sbuf_tensor("ostat", [128, 2, 512], F32))
                yo = ms.enter_context(nc.sbuf_tensor("yo", [128, 4, KT, 512], F32))
                r_sq = [Res(f"osq{k}") for k in range(4)]
                r_stat = [Res(f"ostat{k}") for k in range(2)]
                r_yo = [[Res(f"yo{s}_{k}") for k in range(KT)] for s in range(4)]
                toks = []
                for i in range(4):
                    t0 = 128 + i * 512
                    s = i

                    def outfn(k, s=s):
                        return yo[:, s, k, :]
                    rms_tile(lambda k: xs(k, t0, 512), lambda k: xres(k, t0, 512), 512,
                             lambda k: vecs[:, 0, V_GFIN, k:k + 1], outfn, r_yo[s], sq, r_sq, stat, r_stat, alt_rstd=True)
                    for k in range(KT):
                        toks.append(P.dma("sp", "os", yT[k * 128:(k + 1) * 128, i * 512:(i + 1) * 512],
                                          yo[:, s, k, :], reads=[r_yo[s][k]]))
                P.wait_all("sp", [("os", P.dma_count["os"])])

        for l in range(depth_run):
            if DBG[0] < 20 or DBG[0] >= 30:
                mixer_layer(l)
            if DBG[0] >= 11:
                ffn_layer(l, moe=(l % 2 == 1))
        final()
        stuck, val = P.check_deadlock()
        print("deadlock check:", stuck if stuck else "OK", {n: len(v) for n, v in P.trace.items()}, val)
    return nc


def _vec8(v):
    return np.ascontiguousarray(np.asarray(v, np.float32).reshape(KT, 128).T)


_NC_CACHE = {}


def kernel(x, g_mix, w_in, conv_w, conv_b, conv_ln_g, conv_ln_b, w_conv_out,
           sg_ln_g, sg_ln_b, sg_w, sg_b, w_sg_out, w_o, g_ffn,
           ffn_w1, ffn_w3, ffn_w2, moe_router, moe_w1, moe_w3, moe_w2, g_final, _depth_run=2):
    f32 = np.float32
    x = np.asarray(x, f32)
    vecs = np.zeros((128, 2, NV, KT), f32)
    for l in range(2):
        vecs[:, l, V_GMIX] = _vec8(g_mix[l])
        vecs[:, l, V_CONVB] = _vec8(conv_b[l])
        vecs[:, l, V_CLNG] = _vec8(conv_ln_g[l])
        vecs[:, l, V_CLNB] = _vec8(conv_ln_b[l])
        vecs[:, l, V_SLNG] = _vec8(sg_ln_g[l])
        vecs[:, l, V_SLNB] = _vec8(sg_ln_b[l])
        vecs[:, l, V_GFFN] = _vec8(g_ffn[l])
        vecs[:, l, V_GFIN] = _vec8(g_final)
    cw = np.asarray(conv_w, f32)
    convw = np.ascontiguousarray(cw.transpose(2, 0, 1).reshape(KT, 128, 2, CONV_K).transpose(1, 2, 0, 3))
    sgwT = np.ascontiguousarray(np.asarray(sg_w, f32).transpose(3, 0, 1, 2))
    sgb = np.ascontiguousarray(np.broadcast_to(np.asarray(sg_b, f32)[None], (128, 2, 8, 128)))
    router = np.ascontiguousarray(np.asarray(moe_router, f32)[0].reshape(KT, 128, NE).transpose(1, 0, 2))
    tril = np.triu(np.ones((128, 128), f32))
    ident = np.eye(128, dtype=f32)
    shared = {
        "w_in": np.asarray(w_in, f32), "w_conv_out": np.asarray(w_conv_out, f32),
        "w_sg_out": np.asarray(w_sg_out, f32), "w_o": np.asarray(w_o, f32),
        "ffn_w1": np.asarray(ffn_w1, f32)[0], "ffn_w3": np.asarray(ffn_w3, f32)[0],
        "ffn_w2": np.asarray(ffn_w2, f32)[0],
        "moe_w1": np.asarray(moe_w1, f32)[0], "moe_w3": np.asarray(moe_w3, f32)[0],
        "moe_w2": np.asarray(moe_w2, f32)[0],
        "vecs": vecs, "convw": convw, "sgwT": sgwT, "sgb": sgb, "router": router,
        "tril": tril, "ident": ident,
    }
    if _depth_run < 2:
        for k in ("moe_w1", "moe_w3", "moe_w2"):
            shared.pop(k)
    in_maps = []
    for c in range(NCORES):
        b, hf = c // 2, c % 2
        start = hf * TOK
        xe = np.zeros((2304, D), f32)
        lo = start - 256
        if lo >= 0:
            xe[:] = x[b, lo:start + TOK]
        else:
            xe[256:] = x[b, 0:TOK]
        m = dict(shared)
        m["xT"] = np.ascontiguousarray(xe.T)
        m["cmask"] = np.full((128, 1), 1.0 if hf == 1 else 0.0, f32)
        in_maps.append(m)
    key = (_depth_run, DBG[0], DUMP[0])
    if key not in _NC_CACHE:
        _NC_CACHE[key] = build_nc(depth_run=_depth_run)
    nc = _NC_CACHE[key]
    res = run_bass_kernel_spmd(nc, in_maps, core_ids=list(range(NCORES)))
    LASTRES[0] = res
    out = np.empty((BATCH, SEQ, D), f32)
    for c in range(NCORES):
        b, hf = c // 2, c % 2
        out[b, hf * TOK:(hf + 1) * TOK, :] = res.results[c]["yT"].T
    return out
```

```python
import numpy as np
import concourse.bass as bass
import concourse.mybir as mybir
from concourse.bass_utils import run_bass_kernel_spmd

F32 = mybir.dt.float32
BF16 = mybir.dt.bfloat16
AF = mybir.ActivationFunctionType
ALU = mybir.AluOpType
AX = mybir.AxisListType

D = 1024
KT = 8
SEQ = 4096
BATCH = 4
NCORES = 8
TOK = 2048
NTOK = TOK + 128
CONV_K = 31
FF_DENSE = 2816
FF_EXPERT = 3584
NE = 8
EPS = 1e-6
NSLOT = 6
V_GMIX, V_CONVB, V_CLNG, V_CLNB, V_SLNG, V_SLNB, V_GFFN, V_GFIN = range(8)
NV = 8


class Res:
    __slots__ = ("name", "w", "r")

    def __init__(self, name):
        self.name = name
        self.w = None
        self.r = {}


class Eng:
    def __init__(self, name, handle, sem):
        self.name = name
        self.h = handle
        self.sem = sem
        self.count = 0
        self.known = {}
        self.pending = False


class Prog:
    def __init__(self, nc, sems):
        self.nc = nc
        self.sems = sems
        self.eng = {}
        self.dma_count = {}
        self.trace = {}

    def add_engine(self, name, handle, semname):
        self.eng[name] = Eng(name, handle, semname)

    def _wait(self, e, toks):
        need = {}
        for t in toks:
            if t is None:
                continue
            k, v = t[0], t[1]
            if k == e.sem and (len(t) < 3 or e.name == "pe"):
                continue
            if e.known.get(k, 0) >= v:
                continue
            if need.get(k, 0) < v:
                need[k] = v
        for k, v in need.items():
            e.h.wait_ge(self.sems[k], v)
            e.known[k] = v
            self.trace.setdefault(e.name, []).append(("wait", k, v))

    def _deps(self, reads, writes):
        toks = []
        for r in reads:
            if r.w is not None:
                toks.append((r.w[0], r.w[1], "raw"))
        for w in writes:
            toks.append(w.w)
            for k, v in w.r.items():
                toks.append((k, v))
        return toks

    def _commit(self, tok, reads, writes):
        for r in reads:
            k, v = tok
            if r.r.get(k, 0) < v:
                r.r[k] = v
        for w in writes:
            w.w = tok
            w.r = {}

    def op(self, ename, fn, reads=(), writes=(), sig=True):
        e = self.eng[ename]
        self._wait(e, self._deps(reads, writes))
        ins = fn(e.h)
        if sig:
            e.count += 1
            ins.then_inc(self.sems[e.sem], 1)
            self.trace.setdefault(e.name, []).append(("inc", e.sem, 1))
            e.pending = False
            tok = (e.sem, e.count)
        else:
            e.pending = True
            tok = (e.sem, e.count + 1)
        self._commit(tok, reads, writes)
        return ins

    def dma(self, qname, semname, out, in_, reads=(), writes=()):
        e = self.eng[qname]
        self._wait(e, self._deps(reads, writes))
        e.h.dma_start(out=out, in_=in_).then_inc(self.sems[semname], 16)
        self.trace.setdefault(e.name, []).append(("inc", semname, 16))
        c = self.dma_count.get(semname, 0) + 16
        self.dma_count[semname] = c
        tok = (semname, c)
        self._commit(tok, reads, writes)
        return tok

    def dma_multi(self, qname, semname, pairs, reads=(), writes=()):
        e = self.eng[qname]
        self._wait(e, self._deps(reads, writes))
        for out, in_ in pairs:
            e.h.dma_start(out=out, in_=in_).then_inc(self.sems[semname], 16)
            self.trace.setdefault(e.name, []).append(("inc", semname, 16))
        c = self.dma_count.get(semname, 0) + 16 * len(pairs)
        self.dma_count[semname] = c
        tok = (semname, c)
        self._commit(tok, reads, writes)
        return tok

    def retoken(self, semname, ress):
        tok = (semname, self.dma_count[semname])
        for r in ress:
            r.w = tok

    def barrier(self, names=("pe", "act", "dve")):
        for n in names:
            assert not self.eng[n].pending, n
        for n in names:
            e = self.eng[n]
            toks = [(self.eng[m].sem, self.eng[m].count) for m in names if m != n]
            self._wait(e, toks)

    def check_deadlock(self):
        pos = {n: 0 for n in self.trace}
        val = {}
        progress = True
        while progress:
            progress = False
            for n, ev in self.trace.items():
                while pos[n] < len(ev):
                    kind, k, v = ev[pos[n]]
                    if kind == "wait":
                        if val.get(k, 0) < v:
                            break
                    else:
                        val[k] = val.get(k, 0) + v
                    pos[n] += 1
                    progress = True
        stuck = {n: (pos[n], len(ev), ev[pos[n]]) for n, ev in self.trace.items() if pos[n] < len(ev)}
        return stuck, val

    def wait_all(self, ename, toks):
        self._wait(self.eng[ename], toks)


DBG = [99]
DUMP = [0]
LASTRES = [None]


def build_nc(depth_run=2, debug_out=None):
    nc = bass.Bass("TRN2", target_bir_lowering=False)
    dt = nc.dram_tensor
    xT = dt("xT", [D, 2304], F32, kind="ExternalInput").ap()
    w_in = dt("w_in", [2, D, 6144], F32, kind="ExternalInput").ap()
    w_co = dt("w_conv_out", [2, D, D], F32, kind="ExternalInput").ap()
    w_so = dt("w_sg_out", [2, D, D], F32, kind="ExternalInput").ap()
    w_o = dt("w_o", [2, D, D], F32, kind="ExternalInput").ap()
    f_w1 = dt("ffn_w1", [D, FF_DENSE], F32, kind="ExternalInput").ap()
    f_w3 = dt("ffn_w3", [D, FF_DENSE], F32, kind="ExternalInput").ap()
    f_w2 = dt("ffn_w2", [FF_DENSE, D], F32, kind="ExternalInput").ap()
    if depth_run >= 2:
        m_w1 = dt("moe_w1", [NE, D, FF_EXPERT], F32, kind="ExternalInput").ap()
        m_w3 = dt("moe_w3", [NE, D, FF_EXPERT], F32, kind="ExternalInput").ap()
        m_w2 = dt("moe_w2", [NE, FF_EXPERT, D], F32, kind="ExternalInput").ap()
    vecs_d = dt("vecs", [128, 2, NV, KT], F32, kind="ExternalInput").ap()
    convw_d = dt("convw", [128, 2, KT, CONV_K], F32, kind="ExternalInput").ap()
    sgwT_d = dt("sgwT", [128, 2, 8, 128], F32, kind="ExternalInput").ap()
    sgb_d = dt("sgb", [128, 2, 8, 128], F32, kind="ExternalInput").ap()
    router_d = dt("router", [128, KT, NE], F32, kind="ExternalInput").ap()
    tril_d = dt("tril", [128, 128], F32, kind="ExternalInput").ap()
    ident_d = dt("ident", [128, 128], F32, kind="ExternalInput").ap()
    cmask_d = dt("cmask", [128, 1], F32, kind="ExternalInput").ap()
    yT = dt("yT", [D, TOK], F32, kind="ExternalOutput").ap()

    from contextlib import ExitStack
    with ExitStack() as es:
        def sb(name, shape, dtype):
            return es.enter_context(nc.sbuf_tensor(name, shape, dtype))

        semnames = ["pe", "act", "dve", "cs", "xs", "os", "x0s", "sg0", "sg1"] + [f"xb{k}" for k in range(KT)] + [f"xc{k}" for k in range(KT)] + [f"ws{i}" for i in range(NSLOT)]
        sems = {n: es.enter_context(nc.semaphore(n)) for n in semnames}
        P = Prog(nc, sems)
        P.add_engine("pe", nc.tensor, "pe")
        P.add_engine("act", nc.scalar, "act")
        P.add_engine("dve", nc.vector, "dve")
        P.add_engine("sp", nc.sync, None)
        P.add_engine("pool", nc.gpsimd, None)

        x_all = sb("x_all", [128, KT, NTOK], F32)
        ring = sb("ring", [128, NSLOT, 4096], BF16)
        vecs = sb("vecs_sb", [128, 2, NV, KT], F32)
        convw = sb("convw_sb", [128, 2, KT, CONV_K], F32)
        sgw_f = sb("sgw_f", [128, 8, 128], F32)
        sgb = sb("sgb_sb", [128, 8, 128], F32)
        router = sb("router_sb", [128, KT, NE], F32)
        tril = sb("tril_sb", [128, 128], F32)
        ident = sb("ident_sb", [128, 128], F32)
        cmask = sb("cmask_sb", [128, 1], F32)
        ones_bf = sb("ones_bf", [128, 128], BF16)
        ones_f = sb("ones_f", [128, 128], F32)
        onecol = sb("onecol", [128, 1], F32)
        epscol = sb("epscol", [128, 1], F32)
        ident_bf = sb("ident_bf", [128, 128], BF16)
        sgwT = sb("sgwT_bf", [128, 8, 128], BF16)
        Cg = sb("Cg", [128, 8, 128], F32)

        psum = [es.enter_context(nc.psum_tensor(f"ps{i}", [128, 512], F32)) for i in range(8)]
        ps_res = [Res(f"ps{i}") for i in range(8)]
        ps_half = [[Res(f"ps{i}h{h}") for h in range(2)] for i in range(8)]
        bank_ctr = [0]

        def bres(b):
            return [ps_half[b][0], ps_half[b][1]]

        def next_bank(excl=()):
            while True:
                b = bank_ctr[0] % 8
                bank_ctr[0] += 1
                if b not in excl:
                    return b

        r_x = [[Res(f"x{k}_{c}") for c in range(17)] for k in range(KT)]
        r_const = Res("const")
        r_sgwT = Res("sgwT")
        r_Cg = Res("Cg")
        slot_res = [Res(f"slot{i}") for i in range(NSLOT)]
        panel_ctr = [0]

        r_sgin = Res("sgin")
        cl = [(vecs, vecs_d), (convw, convw_d), (router, router_d),
              (tril, tril_d), (ident, ident_d), (cmask, cmask_d)]
        for t, d_ in cl:
            P.dma("sp", "cs", t[:], d_, writes=[r_const])
        P.retoken("cs", [r_const])
        def load_x():
            P.dma("sp", "xs", x_all[:, :, 0:128], xT[:, 128:256].rearrange("(k p) c -> p k c", p=128),
                  writes=[r_x[k][0] for k in range(KT)])
            for k in range(KT):
                P.dma("sp", f"xb{k}", x_all[:, k, 128:640], xT[k * 128:(k + 1) * 128, 256:768],
                      writes=[r_x[k][c] for c in range(1, 5)])

        def load_x_rest():
            for k in range(KT):
                P.dma("pool", f"xc{k}", x_all[:, k, 640:NTOK], xT[k * 128:(k + 1) * 128, 768:2304],
                      writes=[r_x[k][c] for c in range(5, 17)])
        r_ones = Res("ones")
        P.op("dve", lambda e: e.memset(ones_bf[:], 1.0), writes=[r_ones])
        P.op("dve", lambda e: e.memset(ones_f[:], 1.0), writes=[r_ones])
        P.op("dve", lambda e: e.memset(onecol[:], 1.0), writes=[r_ones])
        P.op("dve", lambda e: e.memset(epscol[:], EPS), writes=[r_ones])
        P.op("dve", lambda e: e.tensor_copy(out=ident_bf[:], in_=ident[:]), reads=[r_const], writes=[r_ones])

        dumps = []

        def dump(name, ap, shape, dtype, reads):
            if not DUMP[0]:
                return
            d_ = nc.dram_tensor("dbg_" + name, list(shape), dtype, kind="ExternalOutput").ap()
            P.dma("sp", "os", d_, ap, reads=reads)
            dumps.append(name)

        def xs(k, t0, n):
            return x_all[:, k, t0:t0 + n]

        def xres(k, t0, n):
            return [r_x[k][c] for c in range(t0 // 128, (t0 + n) // 128)]

        def load_panel(src2d, kt, cols):
            i = panel_ctr[0] % NSLOT
            panel_ctr[0] += 1
            dst = ring[:, i, 0:kt * cols].rearrange("p (k n) -> p k n", k=kt)
            srcv = src2d.rearrange("(k p) n -> p k n", p=128)
            pairs = []
            for c0 in range(0, cols, 512):
                c1 = min(cols, c0 + 512)
                pairs.append((dst[:, :, c0:c1], srcv[:, :, c0:c1]))
            P.dma_multi("pool", f"ws{i}", pairs, writes=[slot_res[i]])
            return dst, slot_res[i]

        rms_ctr = [0]

        def rms_tile(src_fn, src_res_fn, n, gcol_fn, out_fn, out_res, sq, r_sq, stat, r_stat, extra_f32=None,
                     alt_rstd=False):
            b = next_bank()
            for k in range(KT):
                s = k % 2
                P.op("act", lambda e, k=k, s=s: e.activation(out=sq[:, s, 0:n], in_=src_fn(k), func=AF.Square),
                     reads=src_res_fn(k), writes=[r_sq[s]])
                P.op("pe", lambda e, k=k, s=s: e.matmul(psum[b][:, 0:n], lhsT=ones_bf[:], rhs=sq[:, s, 0:n],
                                                        start=(k == 0), stop=(k == KT - 1)),
                     reads=[r_sq[s], r_ones], writes=bres(b), sig=True)
            rslot = rms_ctr[0] % 2 if alt_rstd else 0
            rms_ctr[0] += 1
            rstd = stat[:, rslot, 0:n]
            P.op("act", lambda e: e.activation(out=rstd, in_=psum[b][:, 0:n], func=AF.Sqrt, bias=epscol[:, 0:1],
                                               scale=1.0 / D),
                 reads=bres(b) + [r_ones], writes=[r_stat[rslot]])
            P.op("dve", lambda e: e.reciprocal(out=rstd, in_=rstd),
                 reads=[r_stat[rslot]], writes=[r_stat[rslot]])
            for k in range(KT):
                P.op("dve", lambda e, k=k: e.scalar_tensor_tensor(out=out_fn(k), in0=src_fn(k), scalar=gcol_fn(k),
                                                                  in1=rstd, op0=ALU.mult, op1=ALU.mult),
                     reads=src_res_fn(k) + [r_stat[rslot], r_const], writes=[out_res[k]])
                if extra_f32 is not None:
                    ef, er = extra_f32
                    P.op("dve", lambda e, k=k: e.scalar_tensor_tensor(out=ef(k), in0=src_fn(k), scalar=gcol_fn(k),
                                                                      in1=rstd, op0=ALU.mult, op1=ALU.mult),
                         reads=src_res_fn(k) + [r_stat[rslot], r_const], writes=[er[k]])
            return rslot

        def mm_cols(bank, n, panel, pres, col0, rhs_fn, rhs_res, kt=KT, sig_last=True):
            for k in range(kt):
                P.op("pe", lambda e, k=k: e.matmul(psum[bank][:, 0:n], lhsT=panel[:, k, col0:col0 + 128],
                                                   rhs=rhs_fn(k), start=(k == 0), stop=(k == kt - 1)),
                     reads=[pres] + rhs_res(k), writes=bres(bank), sig=(sig_last and k == kt - 1))

        def mixer_layer(l):
            with ExitStack() as ms:
                def msb(name, shape, dtype):
                    return ms.enter_context(nc.sbuf_tensor(f"{name}_{l}", shape, dtype))
                h = msb("h", [128, KT, 512], BF16)
                a_ext = msb("a_ext", [128, KT, 30 + 512], BF16)
                cv = msb("cv", [128, KT, 512], F32)
                ma = msb("ma", [128, KT, 512], BF16)
                gu = msb("gu", [128, KT, 512], BF16)
                sq = msb("sq", [128, 4, 512], BF16)
                sg = msb("sg", [128, 2, 512], F32)
                stat = msb("stat", [128, 4, 512], F32)
                small = msb("small", [128, 16, 8], F32)
                dgA = msb("dgA", [128, 16, 128], BF16)
                dgB = msb("dgB", [128, 16, 128], BF16)
                r_dgA = Res("dgA")
                r_dgB = Res("dgB")
                r_h = [Res(f"h{k}") for k in range(KT)]
                r_a = [Res(f"a{k}") for k in range(KT)]
                r_cv = [Res(f"cv{k}") for k in range(KT)]
                r_ma = [Res(f"ma{k}") for k in range(KT)]
                r_gu = [Res(f"gu{k}") for k in range(KT)]
                r_sq = [Res(f"sq{k}") for k in range(4)]
                r_sg = [Res(f"sg{k}") for k in range(2)]
                r_stat = [Res(f"stat{k}") for k in range(4)]
                x0 = cv[:, 0:2, :].rearrange("p a (b c) -> p (a b) c", c=128)
                r_x0 = [r_cv[0]] * 4 + [r_cv[1]] * 4
                r_small = Res("small")
                sg_ctr = [0]
                gvs = [cv[:, 0:2, :].rearrange("p a b -> p (a b)"),
                       cv[:, 2:4, :].rearrange("p a b -> p (a b)")]
                gv = gvs[0]
                sqv = dgA[:, :, :].rearrange("p a b -> p (a b)").bitcast(F32)
                vn = cv[:, 4:8, :].rearrange("p a b -> p (a b)").bitcast(BF16).rearrange("p (c n) -> p c n", c=4)
                r_gvs = [[r_cv[0], r_cv[1]], [r_cv[2], r_cv[3]]]
                r_gv = r_gvs[0]
                r_sqv = [r_dgA]

                def vcol(idx, j):
                    return vecs[:, l, idx, j:j + 1]

                P.dma_multi("sp", f"sg{l}", [(sgw_f[:], sgwT_d[:, l, :, :]), (sgb[:], sgb_d[:, l, :, :])],
                            writes=[r_sgin])
                P.op("dve", lambda e: e.tensor_tensor(out=sgwT[:], in0=sgw_f[:, :, :],
                                                      in1=tril[:].unsqueeze(1).to_broadcast([128, 8, 128]),
                                                      op=ALU.mult),
                     reads=[r_const, r_sgin], writes=[r_sgwT])
                for half in range(2):
                    b = next_bank()
                    P.op("pe", lambda e, half=half, b=b: e.matmul(
                        psum[b][:, :], lhsT=ones_bf[:],
                        rhs=sgwT[:, half * 4:(half + 1) * 4, :].rearrange("p g t -> p (g t)"),
                        start=True, stop=True), reads=[r_sgwT, r_ones], writes=bres(b))
                    for gg in range(4):
                        g = half * 4 + gg
                        P.op("dve", lambda e, g=g, gg=gg, b=b: e.scalar_tensor_tensor(
                            out=Cg[:, g, :], in0=psum[b][:, gg * 128:(gg + 1) * 128], scalar=vcol(V_SLNB, g),
                            in1=sgb[:, g, :], op0=ALU.mult, op1=ALU.add),
                            reads=bres(b) + [r_const, r_sgin], writes=[r_Cg])

                if l == 0:
                    load_x()

                h_ready = [None]

                def m1(t0, n, use_x0=False):
                    if use_x0:
                        src_fn = lambda k: x0[:, k, 0:n]
                        src_res = lambda k: [r_x0[k]]
                    else:
                        src_fn = lambda k: xs(k, t0, n)
                        src_res = lambda k: xres(k, t0, n)
                    rms_tile(src_fn, src_res, n, lambda k: vcol(V_GMIX, k), lambda k: h[:, k, 0:n], r_h,
                             sq, r_sq, stat, r_stat)
                    h_ready[0] = (t0, n, use_x0)

                def tile(t0, n, a_only, use_x0=False, masked=False, nxt=None):
                    nch = n // 128
                    if h_ready[0] != (t0, n, use_x0):
                        m1(t0, n, use_x0)
                    h_ready[0] = None
                    hk = lambda k: h[:, k, 0:n]
                    hres = lambda k: [r_h[k]]
                    W = w_in[l]
                    mcol = cmask[:, 0:1] if masked else onecol[:, 0:1]

                    def build_diag(j):
                        P.op("dve", lambda e: e.tensor_tensor(
                            out=dgA[:, 0:16, :], in0=ident_bf[:].unsqueeze(1).to_broadcast([128, 16, 128]),
                            in1=convw[:, l, j, 0:16].unsqueeze(2).to_broadcast([128, 16, 128]), op=ALU.mult),
                            reads=[r_const, r_ones], writes=[r_dgA])
                        P.op("dve", lambda e: e.tensor_tensor(
                            out=dgB[:, 0:15, :], in0=ident_bf[:].unsqueeze(1).to_broadcast([128, 15, 128]),
                            in1=convw[:, l, j, 16:31].unsqueeze(2).to_broadcast([128, 15, 128]), op=ALU.mult),
                            reads=[r_const, r_ones], writes=[r_dgB])
                    if not a_only:
                        build_diag(0)
                    for jj in range(2):
                        pv, rv = load_panel(W[:, jj * 512:(jj + 1) * 512], KT, 512)
                        pg, rg = load_panel(W[:, 1024 + jj * 512:1024 + (jj + 1) * 512], KT, 512)
                        for j4 in range(4):
                            j = jj * 4 + j4
                            b1 = next_bank()
                            b2 = next_bank()
                            mm_cols(b1, n, pv, rv, j4 * 128, hk, hres)
                            mm_cols(b2, n, pg, rg, j4 * 128, hk, hres)
                            s = sg_ctr[0] % 2
                            sg_ctr[0] += 1
                            P.op("act", lambda e, s=s, b2=b2: e.activation(out=sg[:, s, 0:n], in_=psum[b2][:, 0:n],
                                                                          func=AF.Sigmoid),
                                 reads=bres(b2), writes=[r_sg[s]])
                            P.op("dve", lambda e, s=s, b1=b1, j=j: e.scalar_tensor_tensor(
                                out=a_ext[:, j, 30:30 + n], in0=psum[b1][:, 0:n], scalar=mcol, in1=sg[:, s, 0:n],
                                op0=ALU.mult, op1=ALU.mult),
                                reads=bres(b1) + [r_sg[s], r_const, r_ones], writes=[r_a[j]])
                    if a_only:
                        for j in range(KT):
                            P.op("act", lambda e, j=j: e.activation(out=a_ext[:, j, 0:30], in_=a_ext[:, j, n:n + 30],
                                                                    func=AF.Copy),
                                 reads=[r_a[j]], writes=[r_a[j]])
                        return
                    if DBG[0] <= 2:
                        return
                    bmu, bsq = 6, 7
                    pu = ru = None
                    stat_pending = []
                    for j in range(KT):
                        b = next_bank(excl=(bmu, bsq))
                        if j > 0:
                            build_diag(j)
                        for k in range(CONV_K):
                            dgt, rdg, kk = (dgA, r_dgA, k) if k < 16 else (dgB, r_dgB, k - 16)
                            P.op("pe", lambda e, j=j, k=k, kk=kk, dgt=dgt, b=b: e.matmul(
                                psum[b][:, 0:n], lhsT=dgt[:, kk, :], rhs=a_ext[:, j, k:k + n],
                                start=(k == 0), stop=(k == CONV_K - 1)),
                                reads=[rdg, r_a[j]], writes=bres(b), sig=(k == 15 or k == CONV_K - 1))
                        while stat_pending:
                            stat_pending.pop(0)()
                        s0, s1 = (2 * j) % 4, (2 * j + 1) % 4
                        P.op("act", lambda e, j=j, b=b: e.activation(out=cv[:, j, 0:n], in_=psum[b][:, 0:n],
                                                                      func=AF.Identity, bias=vcol(V_CONVB, j),
                                                                      scale=1.0),
                             reads=bres(b) + [r_const], writes=[r_cv[j]])
                        P.op("act", lambda e, j=j, b=b, s0=s0: e.activation(out=sq[:, s0, 0:n], in_=psum[b][:, 0:n],
                                                                            func=AF.Identity,
                                                                            bias=vcol(V_CONVB, j), scale=1.0),
                             reads=bres(b) + [r_const], writes=[r_sq[s0]])
                        P.op("act", lambda e, j=j, b=b, s1=s1: e.activation(out=sq[:, s1, 0:n], in_=psum[b][:, 0:n],
                                                                            func=AF.Square,
                                                                            bias=vcol(V_CONVB, j), scale=1.0),
                             reads=bres(b) + [r_const], writes=[r_sq[s1]])
                        def stat_mm(j=j, s0=s0, s1=s1):
                            P.op("pe", lambda e: e.matmul(psum[bmu][:, 0:n], lhsT=ones_bf[:],
                                                          rhs=sq[:, s0, 0:n], start=(j == 0), stop=(j == KT - 1)),
                                 reads=[r_sq[s0], r_ones], writes=bres(bmu))
                            P.op("pe", lambda e: e.matmul(psum[bsq][:, 0:n], lhsT=ones_bf[:],
                                                          rhs=sq[:, s1, 0:n], start=(j == 0), stop=(j == KT - 1)),
                                 reads=[r_sq[s1], r_ones], writes=bres(bsq))
                        stat_pending.append(stat_mm)
                        P.op("act", lambda e, j=j: e.activation(out=a_ext[:, j, 0:30], in_=a_ext[:, j, n:n + 30],
                                                                func=AF.Copy),
                             reads=[r_a[j]], writes=[r_a[j]])
                    if DBG[0] <= 3:
                        return
                    mean = stat[:, 1, 0:n]
                    var = stat[:, 2, 0:n]

                    def ln_head():
                        P.op("dve", lambda e: e.tensor_scalar(out=mean, in0=psum[bmu][:, 0:n], scalar1=1.0 / D,
                                                              scalar2=None, op0=ALU.mult),
                             reads=bres(bmu), writes=[r_stat[1]])
                        P.op("dve", lambda e: e.tensor_tensor(out=var, in0=mean, in1=mean, op=ALU.mult),
                             reads=[r_stat[1]], writes=[r_stat[2]])
                        P.op("dve", lambda e: e.scalar_tensor_tensor(out=var, in0=psum[bsq][:, 0:n], scalar=1.0 / D,
                                                                     in1=var, op0=ALU.mult, op1=ALU.subtract),
                             reads=bres(bsq) + [r_stat[2]], writes=[r_stat[2]])
                        for j in range(KT):
                            P.op("dve", lambda e, j=j: e.tensor_tensor(out=cv[:, j, 0:n], in0=cv[:, j, 0:n],
                                                                       in1=mean, op=ALU.subtract),
                                 reads=[r_cv[j], r_stat[1]], writes=[r_cv[j]])

                    upend = stat_pending
                    for j in range(KT):
                        if j % 4 == 0:
                            pu, ru = load_panel(W[:, 2048 + (j // 4) * 512:2048 + (j // 4 + 1) * 512], KT, 512)
                        bu = next_bank(excl=(bmu, bsq))
                        mm_cols(bu, n, pu, ru, (j % 4) * 128, hk, hres)
                        if j == 0:
                            while upend:
                                upend.pop(0)()
                            ln_head()
                        P.op("act", lambda e, bu=bu, j=j: e.activation(out=gu[:, j, 0:n], in_=psum[bu][:, 0:n],
                                                                      func=AF.Gelu),
                             reads=bres(bu), writes=[r_gu[j]])
                        if j == 0:
                            P.op("act", lambda e: e.activation(out=var, in_=var, func=AF.Sqrt, bias=epscol[:, 0:1],
                                                               scale=1.0),
                                 reads=[r_stat[2], r_ones], writes=[r_stat[2]])
                            P.op("dve", lambda e: e.reciprocal(out=var, in_=var),
                                 reads=[r_stat[2]], writes=[r_stat[2]])
                            for jn in range(KT):
                                P.op("dve", lambda e, jn=jn: e.tensor_tensor(out=cv[:, jn, 0:n], in0=cv[:, jn, 0:n],
                                                                             in1=var, op=ALU.mult),
                                     reads=[r_cv[jn], r_stat[2]], writes=[r_cv[jn]])
                    for j in range(KT):
                        P.op("act", lambda e, j=j: e.activation(out=a_ext[:, j, 30:30 + n], in_=cv[:, j, 0:n],
                                                                func=AF.Silu, bias=vcol(V_CLNB, j),
                                                                scale=vcol(V_CLNG, j)),
                             reads=[r_cv[j], r_const], writes=[r_a[j]])
                    for jj in range(2):
                        pg, rg = load_panel(W[:, 4096 + jj * 512:4096 + (jj + 1) * 512], KT, 512)
                        for j4 in range(4):
                            j = jj * 4 + j4
                            b2 = next_bank(excl=(bmu, bsq))
                            mm_cols(b2, n, pg, rg, j4 * 128, hk, hres)
                            P.op("act", lambda e, b2=b2, j=j: e.activation(out=ma[:, j, 0:n], in_=psum[b2][:, 0:n],
                                                                          func=AF.Sigmoid),
                                 reads=bres(b2), writes=[r_ma[j]])
                    cak = lambda k: a_ext[:, k, 30:30 + n]
                    cares = lambda k: [r_a[k]]
                    if DBG[0] <= 4:
                        return
                    for jj in range(2):
                        pc, rc = load_panel(w_co[l][:, jj * 512:(jj + 1) * 512], KT, 512)
                        for j4 in range(4):
                            j = jj * 4 + j4
                            b1 = next_bank()
                            mm_cols(b1, n, pc, rc, j4 * 128, cak, cares)
                            P.op("dve", lambda e, b1=b1, j=j: e.tensor_tensor(
                                out=ma[:, j, 0:n], in0=psum[b1][:, 0:n], in1=ma[:, j, 0:n], op=ALU.mult),
                                reads=bres(b1) + [r_ma[j]], writes=[r_ma[j]])
                    if DBG[0] <= 5:
                        return
                    if t0 == 128 and l == 0:
                        dump("h", h[:], [128, KT, 512], BF16, r_h)
                        dump("ca", a_ext[:], [128, KT, 542], BF16, r_a)
                        dump("ma1", ma[:], [128, KT, 512], BF16, r_ma)
                        dump("gu", gu[:], [128, KT, 512], BF16, r_gu)
                    if DBG[0] <= 6:
                        return
                    pv0, rv0 = load_panel(W[:, 3072:3584], KT, 512)
                    pv1, rv1 = load_panel(W[:, 3584:4096], KT, 512)
                    def v_stage_a(c):
                        gvc, r_gvc, so = gvs[c % 2], r_gvs[c % 2], 8 * (c % 2)
                        for half, (pv, rv) in enumerate(((pv0, rv0), (pv1, rv1))):
                            b1 = next_bank()
                            for k in range(KT):
                                P.op("pe", lambda e, k=k, b1=b1, pv=pv: e.matmul(
                                    psum[b1][:, :], lhsT=h[:, k, c * 128:(c + 1) * 128], rhs=pv[:, k, :],
                                    start=(k == 0), stop=(k == KT - 1)),
                                    reads=[rv, r_h[k]], writes=bres(b1), sig=(k == KT - 1))
                            P.op("act", lambda e, b1=b1, half=half: e.activation(
                                out=gvc[:, half * 512:(half + 1) * 512], in_=psum[b1][:, :], func=AF.Gelu),
                                reads=bres(b1), writes=[r_gvc[half]])
                        P.op("act", lambda e: e.activation(out=sqv, in_=gvc, func=AF.Square),
                             reads=r_gvc, writes=r_sqv)
                        s1 = small[:, so + 0, :]
                        s2 = small[:, so + 1, :]
                        mu = small[:, so + 2, :]
                        rs = small[:, so + 3, :]
                        r_sm = r_smalls[c % 2]
                        P.op("dve", lambda e: e.tensor_reduce(out=s1, in_=gvc.rearrange("p (g d) -> p g d", g=8),
                                                              axis=AX.X, op=ALU.add),
                             reads=r_gvc, writes=[r_sm])
                        P.op("dve", lambda e: e.tensor_reduce(out=s2, in_=sqv.rearrange("p (g d) -> p g d", g=8),
                                                              axis=AX.X, op=ALU.add),
                             reads=r_sqv, writes=[r_sm])
                        P.op("dve", lambda e: e.tensor_scalar(out=mu, in0=s1, scalar1=1.0 / 128, scalar2=None,
                                                              op0=ALU.mult), reads=[r_sm], writes=[r_sm])
                        P.op("dve", lambda e: e.tensor_tensor(out=rs, in0=mu, in1=mu, op=ALU.mult),
                             reads=[r_sm], writes=[r_sm])
                        P.op("dve", lambda e: e.scalar_tensor_tensor(out=rs, in0=s2, scalar=1.0 / 128, in1=rs,
                                                                     op0=ALU.mult, op1=ALU.subtract),
                             reads=[r_sm], writes=[r_sm])

                    def v_stage_b(c):
                        gvc, r_gvc, so = gvs[c % 2], r_gvs[c % 2], 8 * (c % 2)
                        mu = small[:, so + 2, :]
                        rs = small[:, so + 3, :]
                        nb = small[:, so + 4, :]
                        r_sm = r_smalls[c % 2]
                        P.op("act", lambda e: e.activation(out=rs, in_=rs, func=AF.Sqrt, bias=epscol[:, 0:1],
                                                           scale=1.0),
                             reads=[r_sm, r_ones], writes=[r_sm])
                        P.op("dve", lambda e: e.reciprocal(out=rs, in_=rs),
                             reads=[r_sm], writes=[r_sm])
                        P.op("dve", lambda e: e.scalar_tensor_tensor(out=nb, in0=mu, scalar=-1.0, in1=rs,
                                                                     op0=ALU.mult, op1=ALU.mult),
                             reads=[r_sm], writes=[r_sm])
                        P.op("dve", lambda e: e.tensor_tensor(
                            out=sqv.rearrange("p (g d) -> p g d", g=8), in0=gvc.rearrange("p (g d) -> p g d", g=8),
                            in1=rs.unsqueeze(2).to_broadcast([128, 8, 128]), op=ALU.mult),
                            reads=r_gvc + [r_sm], writes=r_sqv)
                        P.op("dve", lambda e: e.tensor_tensor(
                            out=vn[:, c, :].rearrange("p (g d) -> p g d", g=8),
                            in0=sqv.rearrange("p (g d) -> p g d", g=8),
                            in1=nb.unsqueeze(2).to_broadcast([128, 8, 128]), op=ALU.add),
                            reads=r_sqv + [r_sm], writes=[r_cv[4 + c]])

                    r_smalls = [Res("small0"), Res("small1")]
                    for c in range(nch + 1):
                        if c < nch:
                            v_stage_a(c)
                        if c >= 1:
                            v_stage_b(c - 1)
                    for jj in range(2):
                        pg, rg = load_panel(W[:, 5120 + jj * 512:5120 + (jj + 1) * 512], KT, 512)
                        for j4 in range(4):
                            j = jj * 4 + j4
                            b2 = next_bank()
                            mm_cols(b2, n, pg, rg, j4 * 128, hk, hres)
                            P.op("act", lambda e, b2=b2, j=j: e.activation(out=a_ext[:, j, 30:30 + n],
                                                                          in_=psum[b2][:, 0:n], func=AF.Sigmoid),
                                 reads=bres(b2), writes=[r_a[j]])
                    if t0 == 128 and l == 0:
                        dump("vn", vn, [128, 4, 1024], BF16, r_cv[4:8])
                        dump("gv", gv, [128, 1024], F32, r_gv)
                        dump("small", small[:], [128, 8, 8], F32, [r_small])
                    if DBG[0] <= 7:
                        return
                    for g in range(8):
                        b1 = next_bank()
                        for c in range(nch):
                            P.op("pe", lambda e, g=g, c=c, b1=b1: e.matmul(
                                psum[b1][:, c * 128:(c + 1) * 128], lhsT=vn[:, c, g * 128:(g + 1) * 128],
                                rhs=sgwT[:, g, :], start=True, stop=True),
                                reads=[r_cv[4 + c], r_sgwT], writes=bres(b1), sig=(c == nch - 1))
                        tmp = stat[:, 3, 0:n]
                        P.op("dve", lambda e, g=g, b1=b1: e.scalar_tensor_tensor(
                            out=tmp.rearrange("p (c t) -> p c t", c=nch),
                            in0=psum[b1][:, 0:n].rearrange("p (c t) -> p c t", c=nch),
                            scalar=vcol(V_SLNG, g),
                            in1=Cg[:, g, :].unsqueeze(1).to_broadcast([128, nch, 128]),
                            op0=ALU.mult, op1=ALU.add),
                            reads=bres(b1) + [r_Cg, r_const], writes=[r_stat[3]])
                        P.op("dve", lambda e, g=g: e.tensor_tensor(out=gu[:, g, 0:n], in0=gu[:, g, 0:n], in1=tmp,
                                                                   op=ALU.mult),
                             reads=[r_gu[g], r_stat[3]], writes=[r_gu[g]])
                    yk = lambda k: gu[:, k, 0:n]
                    yres = lambda k: [r_gu[k]]
                    if t0 == 128 and l == 0:
                        dump("y", gu[:], [128, KT, 512], BF16, r_gu)
                        dump("Cg", Cg[:], [128, 8, 128], F32, [r_Cg])
                    if DBG[0] <= 8:
                        return
                    for jj in range(2):
                        pc, rc = load_panel(w_so[l][:, jj * 512:(jj + 1) * 512], KT, 512)
                        for j4 in range(4):
                            j = jj * 4 + j4
                            b1 = next_bank()
                            mm_cols(b1, n, pc, rc, j4 * 128, yk, yres)
                            s = sg_ctr[0] % 2
                            sg_ctr[0] += 1
                            P.op("dve", lambda e, s=s, b1=b1, j=j: e.tensor_tensor(
                                out=sg[:, s, 0:n], in0=psum[b1][:, 0:n], in1=a_ext[:, j, 30:30 + n], op=ALU.mult),
                                reads=bres(b1) + [r_a[j]], writes=[r_sg[s]])
                            P.op("dve", lambda e, s=s, j=j: e.tensor_tensor(
                                out=ma[:, j, 0:n], in0=ma[:, j, 0:n], in1=sg[:, s, 0:n], op=ALU.add),
                                reads=[r_ma[j], r_sg[s]], writes=[r_ma[j]])
                    if t0 == 128 and l == 0:
                        dump("m", ma[:], [128, KT, 512], BF16, r_ma)
                    if DBG[0] <= 9:
                        return
                    if nxt is not None:
                        m1(nxt[0], nxt[1])
                    mk = lambda k: ma[:, k, 0:n]
                    mres = lambda k: [r_ma[k]]
                    for jj in range(2):
                        po, ro = load_panel(w_o[l][:, jj * 512:(jj + 1) * 512], KT, 512)
                        for j4 in range(4):
                            j = jj * 4 + j4
                            b1 = next_bank()
                            mm_cols(b1, n, po, ro, j4 * 128, mk, mres)
                            P.op("dve", lambda e, b1=b1, j=j: e.tensor_tensor(
                                out=xs(j, t0, n), in0=psum[b1][:, 0:n], in1=xs(j, t0, n), op=ALU.add),
                                reads=bres(b1) + xres(j, t0, n), writes=xres(j, t0, n))

                if l == 0:
                    for j in range(KT):
                        P.op("dve", lambda e, j=j: e.memset(a_ext[:, j, 0:30], 0.0), writes=[r_a[j]])
                    tile(0, 128, False, nxt=(128, 512))
                    load_x_rest()
                else:
                    tile(0, 128, True, masked=True)
                for i in range(4):
                    tile(128 + i * 512, 512, False, nxt=((128 + (i + 1) * 512, 512) if i < 3 else None))
                P.barrier()

        def ffn_layer(l, moe):
            with ExitStack() as ms:
                def msb(name, shape, dtype):
                    return ms.enter_context(nc.sbuf_tensor(f"f{name}_{l}", shape, dtype))
                h2 = msb("h2", [128, KT, NTOK], BF16)
                sq = msb("sq", [128, 4, 256], BF16)
                stat = msb("stat", [128, 2, 256], F32)
                ssl = msb("ssl", [128, 4, 256], F32)
                act = msb("act", [128, 8, 256], BF16)
                r_h2 = [[Res(f"h2_{k}_{c}") for c in range(17)] for k in range(KT)]
                r_sq = [Res(f"fsq{k}") for k in range(4)]
                r_stat = [Res(f"fstat{k}") for k in range(2)]
                r_ssl = [Res(f"ssl{k}") for k in range(4)]
                r_act = [Res(f"act{k}") for k in range(8)]
                if moe:
                    diagbuf = msb("diagbuf", [128, TOK], F32)
                    router_g = msb("router_g", [128, KT, NE], F32)
                    rtok = msb("rtok", [128, 16], F32)
                    r_rg = Res("router_g")
                    r_rtok = Res("rtok")
                    comb_b = msb("comb_b", [128, TOK], F32)
                    L = msb("L", [128, 16, NE], F32)
                    L2 = msb("L2", [128, 16, NE], F32)
                    comb = msb("comb", [128, 16, NE], F32)
                    m12 = msb("m12", [128, 4, 16], F32)
                    r_h2f = [Res("diagbuf")]
                    r_combb = Res("comb_b")
                    r_L = Res("L")
                    diag = diagbuf[:, :]
                    P.op("dve", lambda e: e.tensor_tensor(
                        out=router_g[:], in0=router[:],
                        in1=vecs[:, l, V_GFFN, :].unsqueeze(2).to_broadcast([128, KT, NE]), op=ALU.mult),
                        reads=[r_const], writes=[r_rg])
                tiles = ([] if moe else [(0, 128)]) + [(128 + 256 * i, 256) for i in range(8)]

                for (t0, n) in tiles:
                    out_res = [Res("tmp") for _ in range(KT)]
                    rslot = rms_tile(lambda k: xs(k, t0, n), lambda k: xres(k, t0, n), n,
                                     lambda k: vecs[:, l, V_GFFN, k:k + 1],
                                     lambda k: h2[:, k, t0:t0 + n], out_res, sq, r_sq, stat, r_stat, alt_rstd=True)
                    for k in range(KT):
                        for c in range(t0 // 128, (t0 + n) // 128):
                            r_h2[k][c].w = out_res[k].w
                    if moe:
                        for cc in range(n // 128):
                            c = (t0 - 128) // 128 + cc
                            tc0 = t0 + cc * 128
                            b = next_bank()
                            for k in range(KT):
                                P.op("pe", lambda e, k=k, tc0=tc0, b=b: e.matmul(
                                    psum[b][:, 0:NE], lhsT=x_all[:, k, tc0:tc0 + 128], rhs=router_g[:, k, :],
                                    start=(k == 0), stop=(k == KT - 1)),
                                    reads=xres(k, tc0, 128) + [r_rg], writes=bres(b), sig=(k == KT - 1))
                            P.op("pe", lambda e, cc=cc, b=b, rslot=rslot: e.matmul(
                                psum[b][:, NE:NE + 1], lhsT=stat[:, rslot, cc * 128:(cc + 1) * 128],
                                rhs=ident[:, 0:1], start=True, stop=True),
                                reads=[r_stat[rslot], r_const], writes=bres(b))
                            P.op("act", lambda e, c=c, b=b: e.activation(out=rtok[:, c:c + 1],
                                                                         in_=psum[b][:, NE:NE + 1], func=AF.Copy),
                                 reads=bres(b), writes=[r_rtok])
                            P.op("dve", lambda e, c=c, b=b: e.tensor_scalar(
                                out=L[:, c, :], in0=psum[b][:, 0:NE], scalar1=rtok[:, c:c + 1], scalar2=None,
                                op0=ALU.mult),
                                reads=bres(b) + [r_rtok], writes=[r_L])
                if moe:
                    m1 = m12[:, 0, :]
                    m2 = m12[:, 1, :]
                    den = m12[:, 2, :]

                    def bc(a):
                        return a.unsqueeze(2).to_broadcast([128, 16, NE])
                    ops = [
                        lambda e: e.tensor_reduce(out=m1, in_=L[:], axis=AX.X, op=ALU.max),
                        lambda e: e.tensor_tensor(out=L2[:], in0=L[:], in1=bc(m1), op=ALU.is_equal),
                        lambda e: e.scalar_tensor_tensor(out=L2[:], in0=L2[:], scalar=-1e30, in1=L[:],
                                                         op0=ALU.mult, op1=ALU.add),
                        lambda e: e.tensor_reduce(out=m2, in_=L2[:], axis=AX.X, op=ALU.max),
                        lambda e: e.tensor_tensor(out=L2[:], in0=L[:], in1=bc(m2), op=ALU.is_ge),
                        lambda e: e.tensor_tensor(out=comb[:], in0=L[:], in1=bc(m1), op=ALU.subtract),
                    ]
                    for f in ops:
                        P.op("dve", f, reads=[r_L], writes=[r_L])
                    P.op("act", lambda e: e.activation(out=comb[:], in_=comb[:], func=AF.Exp),
                         reads=[r_L], writes=[r_L])
                    ops = [
                        lambda e: e.tensor_tensor(out=comb[:], in0=comb[:], in1=L2[:], op=ALU.mult),
                        lambda e: e.tensor_reduce(out=den, in_=comb[:], axis=AX.X, op=ALU.add),
                        lambda e: e.reciprocal(out=den, in_=den),
                        lambda e: e.tensor_tensor(out=comb[:], in0=comb[:], in1=bc(den), op=ALU.mult),
                    ]
                    for f in ops:
                        P.op("dve", f, reads=[r_L], writes=[r_L])

                nexp = NE if moe else 1
                FF = FF_EXPERT if moe else FF_DENSE
                chunks = []
                f0 = 0
                while f0 < FF:
                    w = min(512, FF - f0)
                    chunks.append((f0, w))
                    f0 += w
                hslot = [0]
                pending = []
                if DBG[0] == 21:
                    chunks = []
                if DBG[0] == 22:
                    chunks = chunks[:1]
                if DBG[0] == 23:
                    chunks = chunks[-1:]
                if DBG[0] in (24, 25, 26, 27):
                    chunks = chunks[:1]
                    tiles = tiles[1:2]

                def flush_one():
                    if pending:
                        pending.pop(0)()

                for ex in range(nexp):
                    if moe:
                        while pending:
                            flush_one()
                        W1, W3, W2 = m_w1[ex], m_w3[ex], m_w2[ex]
                        P.op("dve", lambda e, ex=ex: e.tensor_tensor(
                            out=diag.rearrange("p (c t) -> p c t", c=16),
                            in0=ident[:].unsqueeze(1).to_broadcast([128, 16, 128]),
                            in1=comb[:, :, ex:ex + 1].to_broadcast([128, 16, 128]), op=ALU.mult),
                            reads=[r_L, r_const] + r_h2f, writes=r_h2f)
                        for q in range(4):
                            b = 4 + q
                            P.op("pe", lambda e, q=q, b=b: e.matmul(psum[b][:, :], lhsT=ones_f[:],
                                                                    rhs=diag[:, q * 512:(q + 1) * 512],
                                                                    start=True, stop=True),
                                 reads=r_h2f + [r_ones], writes=bres(b))
                            P.op("act", lambda e, q=q, b=b: e.activation(out=comb_b[:, q * 512:(q + 1) * 512],
                                                                         in_=psum[b][:, :], func=AF.Copy),
                                 reads=bres(b), writes=[r_combb])
                    else:
                        W1, W3, W2 = f_w1, f_w3, f_w2
                    for (f0, fw) in chunks:
                        nf = fw // 128
                        p1, r1 = load_panel(W1[:, f0:f0 + fw], KT, fw)
                        p3, r3 = load_panel(W3[:, f0:f0 + fw], KT, fw)
                        p2, r2 = load_panel(W2[f0:f0 + fw, :], nf, D)
                        for (t0, n) in tiles:
                            c0, c1 = t0 // 128, (t0 + n) // 128
                            hres = lambda k: [r_h2[k][c] for c in range(c0, c1)]
                            for fi in range(nf):
                                hs = hslot[0] % 4
                                hslot[0] += 1
                                asl = hs + 4 * ((hslot[0] // 4) % 2)
                                bh = 4 + 2 * (hs % 2)
                                bh3 = bh + 1
                                ph1 = psum[bh][:, 0:n]
                                ph3 = psum[bh3][:, 0:n]
                                for k in range(KT):
                                    P.op("pe", lambda e, k=k, fi=fi, ph1=ph1: e.matmul(
                                        ph1, lhsT=p1[:, k, fi * 128:(fi + 1) * 128], rhs=h2[:, k, t0:t0 + n],
                                        start=(k == 0), stop=(k == KT - 1)),
                                        reads=[r1] + hres(k), writes=bres(bh), sig=(k == KT - 1))
                                for k in range(KT):
                                    P.op("pe", lambda e, k=k, fi=fi, ph3=ph3: e.matmul(
                                        ph3, lhsT=p3[:, k, fi * 128:(fi + 1) * 128], rhs=h2[:, k, t0:t0 + n],
                                        start=(k == 0), stop=(k == KT - 1)),
                                        reads=[r3] + hres(k), writes=bres(bh3), sig=(k == KT - 1))
                                if DBG[0] == 25:
                                    continue
                                P.op("act", lambda e, hs=hs, ph1=ph1: e.activation(out=ssl[:, hs, 0:n], in_=ph1,
                                                                                   func=AF.Silu),
                                     reads=bres(bh), writes=[r_ssl[hs]])
                                if moe:
                                    P.op("dve", lambda e, hs=hs: e.tensor_tensor(
                                        out=ssl[:, hs, 0:n], in0=ssl[:, hs, 0:n],
                                        in1=comb_b[:, t0 - 128:t0 - 128 + n], op=ALU.mult),
                                        reads=[r_ssl[hs], r_combb], writes=[r_ssl[hs]])
                                P.op("dve", lambda e, hs=hs, asl=asl, ph3=ph3: e.tensor_tensor(
                                    out=act[:, asl, 0:n], in0=ph3, in1=ssl[:, hs, 0:n], op=ALU.mult),
                                    reads=bres(bh3) + [r_ssl[hs]], writes=[r_act[asl]])
                            if DBG[0] in (25, 26):
                                continue
                            flush_one()
                            asl0 = 4 * (((hslot[0] - 1) // 4) % 2)
                            hs0 = (hslot[0] - nf) % 4

                            def w2_block(n=n, t0=t0, nf=nf, p2=p2, r2=r2, hs0=hs0, hbase=hslot[0] - nf):
                                for bo in range(4):
                                    for ho in range(2):
                                        dtile = 2 * bo + ho
                                        for fi in range(nf):
                                            hs = (hbase + fi) % 4
                                            asl = hs + 4 * (((hbase + fi + 1) // 4) % 2)
                                            P.op("pe", lambda e, fi=fi, asl=asl, dtile=dtile, bo=bo, ho=ho: e.matmul(
                                                psum[bo][:, ho * 256:ho * 256 + n],
                                                lhsT=p2[:, fi, dtile * 128:(dtile + 1) * 128], rhs=act[:, asl, 0:n],
                                                start=(fi == 0), stop=(fi == nf - 1)),
                                                reads=[r2, r_act[asl]], writes=bres(bo),
                                                sig=(fi == nf - 1 and ho == 1))
                                    if DBG[0] == 27:
                                        continue
                                    P.op("dve", lambda e, bo=bo: e.tensor_tensor(
                                        out=x_all[:, 2 * bo:2 * bo + 2, t0:t0 + n],
                                        in0=psum[bo][:, :].rearrange("p (a b) -> p a b", a=2)[:, :, 0:n],
                                        in1=x_all[:, 2 * bo:2 * bo + 2, t0:t0 + n], op=ALU.add),
                                        reads=bres(bo) + xres(2 * bo, t0, n) + xres(2 * bo + 1, t0, n),
                                        writes=xres(2 * bo, t0, n) + xres(2 * bo + 1, t0, n))
                            pending.append(w2_block)
                while pending:
                    flush_one()
                P.barrier()

        def final():
            with ExitStack() as ms:
                sq = ms.enter_context(nc.sbuf_tensor("osq", [128, 4, 512], BF16))
                stat = ms.enter_context(nc.sbuf_tensor("ostat", [128, 2, 512], F32))
                yo = ms.enter_context(nc.sbuf_tensor("yo", [128, 4, KT, 512], F32))
                r_sq = [Res(f"osq{k}") for k in range(4)]
                r_stat = [Res(f"ostat{k}") for k in range(2)]
                r_yo = [[Res(f"yo{s}_{k}") for k in range(KT)] for s in range(4)]
                toks = []
                for i in range(4):
                    t0 = 128 + i * 512
                    s = i

                    def outfn(k, s=s):
                        return yo[:, s, k, :]
                    rms_tile(lambda k: xs(k, t0, 512), lambda k: xres(k, t0, 512), 512,
                             lambda k: vecs[:, 0, V_GFIN, k:k + 1], outfn, r_yo[s], sq, r_sq, stat, r_stat, alt_rstd=True)
                    for k in range(KT):
                        toks.append(P.dma("sp", "os", yT[k * 128:(k + 1) * 128, i * 512:(i + 1) * 512],
                                          yo[:, s, k, :], reads=[r_yo[s][k]]))
                P.wait_all("sp", [("os", P.dma_count["os"])])

        for l in range(depth_run):
            if DBG[0] < 20 or DBG[0] >= 30:
                mixer_layer(l)
            if DBG[0] >= 11:
                ffn_layer(l, moe=(l % 2 == 1))
        final()
        stuck, val = P.check_deadlock()
        print("deadlock check:", stuck if stuck else "OK", {n: len(v) for n, v in P.trace.items()}, val)
    return nc


def _vec8(v):
    return np.ascontiguousarray(np.asarray(v, np.float32).reshape(KT, 128).T)


_NC_CACHE = {}


def kernel(x, g_mix, w_in, conv_w, conv_b, conv_ln_g, conv_ln_b, w_conv_out,
           sg_ln_g, sg_ln_b, sg_w, sg_b, w_sg_out, w_o, g_ffn,
           ffn_w1, ffn_w3, ffn_w2, moe_router, moe_w1, moe_w3, moe_w2, g_final, _depth_run=2):
    f32 = np.float32
    x = np.asarray(x, f32)
    vecs = np.zeros((128, 2, NV, KT), f32)
    for l in range(2):
        vecs[:, l, V_GMIX] = _vec8(g_mix[l])
        vecs[:, l, V_CONVB] = _vec8(conv_b[l])
        vecs[:, l, V_CLNG] = _vec8(conv_ln_g[l])
        vecs[:, l, V_CLNB] = _vec8(conv_ln_b[l])
        vecs[:, l, V_SLNG] = _vec8(sg_ln_g[l])
        vecs[:, l, V_SLNB] = _vec8(sg_ln_b[l])
        vecs[:, l, V_GFFN] = _vec8(g_ffn[l])
        vecs[:, l, V_GFIN] = _vec8(g_final)
    cw = np.asarray(conv_w, f32)
    convw = np.ascontiguousarray(cw.transpose(2, 0, 1).reshape(KT, 128, 2, CONV_K).transpose(1, 2, 0, 3))
    sgwT = np.ascontiguousarray(np.asarray(sg_w, f32).transpose(3, 0, 1, 2))
    sgb = np.ascontiguousarray(np.broadcast_to(np.asarray(sg_b, f32)[None], (128, 2, 8, 128)))
    router = np.ascontiguousarray(np.asarray(moe_router, f32)[0].reshape(KT, 128, NE).transpose(1, 0, 2))
    tril = np.triu(np.ones((128, 128), f32))
    ident = np.eye(128, dtype=f32)
    shared = {
        "w_in": np.asarray(w_in, f32), "w_conv_out": np.asarray(w_conv_out, f32),
        "w_sg_out": np.asarray(w_sg_out, f32), "w_o": np.asarray(w_o, f32),
        "ffn_w1": np.asarray(ffn_w1, f32)[0], "ffn_w3": np.asarray(ffn_w3, f32)[0],
        "ffn_w2": np.asarray(ffn_w2, f32)[0],
        "moe_w1": np.asarray(moe_w1, f32)[0], "moe_w3": np.asarray(moe_w3, f32)[0],
        "moe_w2": np.asarray(moe_w2, f32)[0],
        "vecs": vecs, "convw": convw, "sgwT": sgwT, "sgb": sgb, "router": router,
        "tril": tril, "ident": ident,
    }
    if _depth_run < 2:
        for k in ("moe_w1", "moe_w3", "moe_w2"):
            shared.pop(k)
    in_maps = []
    for c in range(NCORES):
        b, hf = c // 2, c % 2
        start = hf * TOK
        xe = np.zeros((2304, D), f32)
        lo = start - 256
        if lo >= 0:
            xe[:] = x[b, lo:start + TOK]
        else:
            xe[256:] = x[b, 0:TOK]
        m = dict(shared)
        m["xT"] = np.ascontiguousarray(xe.T)
        m["cmask"] = np.full((128, 1), 1.0 if hf == 1 else 0.0, f32)
        in_maps.append(m)
    key = (_depth_run, DBG[0], DUMP[0])
    if key not in _NC_CACHE:
        _NC_CACHE[key] = build_nc(depth_run=_depth_run)
    nc = _NC_CACHE[key]
    res = run_bass_kernel_spmd(nc, in_maps, core_ids=list(range(NCORES)))
    LASTRES[0] = res
    out = np.empty((BATCH, SEQ, D), f32)
    for c in range(NCORES):
        b, hf = c // 2, c % 2
        out[b, hf * TOK:(hf + 1) * TOK, :] = res.results[c]["yT"].T
    return out
```
